# Optimizing a Trainium2 kernel written in Bass

```python
import math
import jax
import jax.numpy as jnp
from jax import lax
import numpy as np

D_MODEL = 1024
BATCH = 16
SEQ = 2048
DEPTH = 2

HEAD_DIM = 64
N_SLOTS = D_MODEL // HEAD_DIM
A_HEADS = 8
A_LATENT = 128
IDX_HEADS = 8
IDX_DIM = 64
A_TOPK_MAX = 256
B_HEADS = 8
B_KV_HEADS = 2
B_WINDOW = 128
C_HEADS = 16
C_KV_HEADS = 2
CMP_BLOCK = 32
CMP_STRIDE = 16
CMP_HIDDEN = 128
SLC_BLOCK = 64
SLC_COUNT = 8
C_WINDOW = 512
MEM_LEN = 256
X_HEADS = 4
X_HEAD_DIM = 128
D_FF = 2816
N_EXPERTS = 8
TOP_K = 2
D_FF_EXPERT = 3584
REL_BUCKETS = 32
REL_MAX_DIST = 128
Q_BLOCK = 128
SLC_Q_BLOCK = 32
LN_EPS = 1e-5
NEG_INF = -1e30
FORCE_BONUS = 1e6
ALPHA = (2 * DEPTH) ** 0.25
BETA = (8 * DEPTH) ** -0.25
N_EVEN = (DEPTH + 1) // 2
N_ODD = DEPTH // 2
EVEN_WIDTHS = (A_HEADS * HEAD_DIM, A_LATENT, IDX_HEADS * IDX_DIM, IDX_DIM, IDX_HEADS,
               B_HEADS * HEAD_DIM, B_KV_HEADS * HEAD_DIM, B_KV_HEADS * HEAD_DIM)
EVEN_COLS = sum(EVEN_WIDTHS)
ODD_WIDTHS = (C_HEADS * HEAD_DIM,) + (C_KV_HEADS * HEAD_DIM,) * 6 + (3 * C_HEADS,)
ODD_COLS = sum(ODD_WIDTHS)

kernel_name = "hybrid_dsa_swa_nsa_moe_deepnorm"


def split_cols(a, widths):
    cuts = [int(c) for c in np.cumsum(widths)[:-1]]
    return jnp.split(a, cuts, axis=-1)


def layer_norm(x, g, b):
    xf = x.astype(jnp.float32)
    mu = jnp.mean(xf, -1, keepdims=True)
    var = jnp.mean(jnp.square(xf - mu), -1, keepdims=True)
    return ((xf - mu) * lax.rsqrt(var + LN_EPS)).astype(x.dtype) * g + b


def rms_norm(x, g):
    xf = x.astype(jnp.float32)
    y = xf * lax.rsqrt(jnp.mean(jnp.square(xf), -1, keepdims=True) + LN_EPS)
    return y.astype(x.dtype) * g


def rel_bucket(dist):
    n = jnp.maximum(dist, 0)
    max_exact = REL_BUCKETS // 2
    nf = jnp.maximum(n, 1).astype(jnp.float32)
    log_b = max_exact + (jnp.log(nf / max_exact) / math.log(REL_MAX_DIST / max_exact)
                         * (REL_BUCKETS - max_exact)).astype(jnp.int32)
    return jnp.where(n < max_exact, n, jnp.minimum(log_b, REL_BUCKETS - 1))


def banded_attention(q, k, v, rel_tbl, window, sinks=None):
    B, S, H, D = q.shape
    kvh = k.shape[2]
    g = H // kvh
    pad = -(-window // Q_BLOCK) * Q_BLOCK
    span = pad + Q_BLOCK
    kp = jnp.pad(k, ((0, 0), (pad, 0), (0, 0), (0, 0)))
    vp = jnp.pad(v, ((0, 0), (pad, 0), (0, 0), (0, 0)))
    dist = np.arange(Q_BLOCK)[:, None] + pad - np.arange(span)[None, :]
    band = jnp.asarray((dist >= 0) & (dist < window))
    bias = rel_tbl.astype(jnp.float32)[rel_bucket(jnp.asarray(dist))]
    bias = bias.transpose(2, 0, 1).reshape(kvh, g, Q_BLOCK, span)
    key_off = jnp.arange(span) - pad

    def block(n):
        t0 = n * Q_BLOCK
        qb = lax.dynamic_slice_in_dim(q, t0, Q_BLOCK, 1).reshape(B, Q_BLOCK, kvh, g, D)
        kb = lax.dynamic_slice_in_dim(kp, t0, span, 1)
        vb = lax.dynamic_slice_in_dim(vp, t0, span, 1)
        logits = jnp.einsum('bqkgd,bskd->bkgqs', qb, kb).astype(jnp.float32) * D ** -0.5 + bias
        ok = band & (t0 + key_off >= 0)[None, :]
        logits = jnp.where(ok, logits, NEG_INF)
        if sinks is None:
            p = jax.nn.softmax(logits, axis=-1)
        else:
            s = sinks.astype(jnp.float32).reshape(kvh, g, 1, 1)
            m = jnp.maximum(jnp.max(logits, -1, keepdims=True), s)
            e = jnp.exp(logits - m)
            p = e / (jnp.sum(e, -1, keepdims=True) + jnp.exp(s - m))
        o = jnp.einsum('bkgqs,bskd->bqkgd', p.astype(v.dtype), vb)
        return o.reshape(B, Q_BLOCK, H, D)

    out = lax.map(block, jnp.arange(S // Q_BLOCK))
    return jnp.moveaxis(out, 0, 1).reshape(B, S, H, D)


def dsa_attention(q_a, c_kv, q_i, k_i, w_i, ckv_gain, w_uk, w_uv, rel_tbl):
    B, S, _ = q_a.shape
    topk = min(A_TOPK_MAX, S // 4)
    q_lat = jnp.einsum('bshd,hdc->bshc', q_a.reshape(B, S, A_HEADS, HEAD_DIM), w_uk)
    c = rms_norm(c_kv, ckv_gain)
    qi = q_i.reshape(B, S, IDX_HEADS, IDX_DIM)
    wi = w_i * IDX_HEADS ** -0.5
    tbl = rel_tbl.astype(jnp.float32)
    kpos = jnp.arange(S)

    def block(n):
        t0 = n * Q_BLOCK
        qpos = t0 + jnp.arange(Q_BLOCK)
        qi_b = lax.dynamic_slice_in_dim(qi, t0, Q_BLOCK, 1)
        wi_b = lax.dynamic_slice_in_dim(wi, t0, Q_BLOCK, 1)
        ql_b = lax.dynamic_slice_in_dim(q_lat, t0, Q_BLOCK, 1)
        act = jax.nn.relu(jnp.einsum('bqhd,bsd->bqhs', qi_b, k_i) * IDX_DIM ** -0.5)
        score = jnp.einsum('bqh,bqhs->bqs', wi_b, act).astype(jnp.float32)
        score = jnp.where(kpos[None, :] <= qpos[:, None], score, NEG_INF)
        _, sel = lax.top_k(score, topk)
        c_sel = jax.vmap(lambda cb, sb: cb[sb])(c, sel)
        dist = qpos[None, :, None] - sel
        bias = tbl[rel_bucket(dist)].transpose(0, 1, 3, 2)
        lg = jnp.einsum('bqhc,bqkc->bqhk', ql_b, c_sel).astype(jnp.float32) * HEAD_DIM ** -0.5 + bias
        lg = jnp.where((dist >= 0)[:, :, None, :], lg, NEG_INF)
        p = jax.nn.softmax(lg, axis=-1).astype(c.dtype)
        o_lat = jnp.einsum('bqhk,bqkc->bqhc', p, c_sel)
        return jnp.einsum('bqhc,hcd->bqhd', o_lat, w_uv)

    out = lax.map(block, jnp.arange(S // Q_BLOCK))
    return jnp.moveaxis(out, 0, 1).reshape(B, S, A_HEADS, HEAD_DIM)


def compress_blocks(kv, pe, w1, w2):
    B, S, G, D = kv.shape
    n_cmp = (S - CMP_BLOCK) // CMP_STRIDE + 1
    idx = np.arange(n_cmp)[:, None] * CMP_STRIDE + np.arange(CMP_BLOCK)[None, :]
    blk = kv[:, idx] + pe[None, None, :, None, :]
    blk = blk.transpose(0, 1, 3, 2, 4).reshape(B, n_cmp, G, CMP_BLOCK * D)
    return jax.nn.gelu(blk @ w1) @ w2


def nsa_attention(q, k_cmp, v_cmp, k_slc, v_slc, k_win, v_win, gate_logits,
                  pe_k, w1_k, w2_k, pe_v, w1_v, w2_v, rel_tbl):
    B, S, H, D = q.shape
    G = C_KV_HEADS
    hpg = H // G
    qg = q.reshape(B, S, G, hpg, D)
    scale = D ** -0.5
    tbl = rel_tbl.astype(jnp.float32)
    pos = np.arange(S)

    n_cmp = (S - CMP_BLOCK) // CMP_STRIDE + 1
    kc = compress_blocks(k_cmp, pe_k, w1_k, w2_k)
    vc = compress_blocks(v_cmp, pe_v, w1_v, w2_v)
    cmp_end = np.arange(n_cmp) * CMP_STRIDE + CMP_BLOCK - 1
    dist_c = pos[:, None] - cmp_end[None, :]
    valid_c = jnp.asarray(dist_c >= 0)
    bias_c = tbl[rel_bucket(jnp.asarray(dist_c))].transpose(2, 0, 1).reshape(G, hpg, S, n_cmp)
    lg = jnp.einsum('bsghd,bngd->bghsn', qg, kc).astype(jnp.float32) * scale + bias_c
    p_cmp = jax.nn.softmax(jnp.where(valid_c, lg, NEG_INF), axis=-1) * valid_c
    o_cmp = jnp.einsum('bghsn,bngd->bsghd', p_cmp.astype(vc.dtype), vc).reshape(B, S, H, D)

    n_slc = S // SLC_BLOCK
    n_sel = min(SLC_COUNT, n_slc)
    cs = np.arange(n_cmp)[:, None] * CMP_STRIDE
    ss = np.arange(n_slc)[None, :] * SLC_BLOCK
    overlap = jnp.asarray(((cs < ss + SLC_BLOCK) & (cs + CMP_BLOCK > ss)).astype(np.float32))
    p_slc = jnp.einsum('bghsn,nj->bgsj', p_cmp, overlap)
    blk = pos // SLC_BLOCK
    jj = np.arange(n_slc)
    forced = (jj[None, :] == 0) | (jj[None, :] == blk[:, None]) | (jj[None, :] == blk[:, None] - 1)
    admissible = jj[None, :] * SLC_BLOCK <= pos[:, None]
    score = jnp.where(jnp.asarray(admissible),
                      p_slc + FORCE_BONUS * jnp.asarray(forced.astype(np.float32)), NEG_INF)
    _, sel = lax.top_k(score, n_sel)

    kb = k_slc.reshape(B, n_slc, SLC_BLOCK, G, D).transpose(0, 3, 1, 2, 4)
    vb = v_slc.reshape(B, n_slc, SLC_BLOCK, G, D).transpose(0, 3, 1, 2, 4)
    bi = jnp.arange(B)[:, None, None, None]
    gi = jnp.arange(G)[None, :, None, None]
    tbl_g = tbl.reshape(REL_BUCKETS, G, hpg).transpose(1, 0, 2)
    in_blk = jnp.arange(SLC_BLOCK)
    n_keys = n_sel * SLC_BLOCK

    def slc_block(n):
        t0 = n * SLC_Q_BLOCK
        qpos = t0 + jnp.arange(SLC_Q_BLOCK)
        qb = lax.dynamic_slice_in_dim(qg, t0, SLC_Q_BLOCK, 1)
        sb = lax.dynamic_slice_in_dim(sel, t0, SLC_Q_BLOCK, 2)
        ks = kb[bi, gi, sb].reshape(B, G, SLC_Q_BLOCK, n_keys, D)
        vs = vb[bi, gi, sb].reshape(B, G, SLC_Q_BLOCK, n_keys, D)
        kpos = (sb[..., None] * SLC_BLOCK + in_blk).reshape(B, G, SLC_Q_BLOCK, n_keys)
        dist = qpos[None, None, :, None] - kpos
        bias = tbl_g[gi, rel_bucket(dist)].transpose(0, 1, 4, 2, 3)
        lg2 = jnp.einsum('bqghd,bgqsd->bghqs', qb, ks).astype(jnp.float32) * scale + bias
        lg2 = jnp.where((dist >= 0)[:, :, None], lg2, NEG_INF)
        p = jax.nn.softmax(lg2, axis=-1).astype(vs.dtype)
        o = jnp.einsum('bghqs,bgqsd->bqghd', p, vs)
        return o.reshape(B, SLC_Q_BLOCK, H, D)

    o_slc = lax.map(slc_block, jnp.arange(S // SLC_Q_BLOCK))
    o_slc = jnp.moveaxis(o_slc, 0, 1).reshape(B, S, H, D)

    o_win = banded_attention(q, k_win, v_win, rel_tbl, C_WINDOW)

    gt = jax.nn.sigmoid(gate_logits).reshape(B, S, H, 3)
    return gt[..., 0:1] * o_cmp + gt[..., 1:2] * o_slc + gt[..., 2:3] * o_win


def even_mixer(x, w_in, ckv_gain, w_uk, w_uv, sinks, w_out, rel_table):
    B, S, _ = x.shape
    q_a, c_kv, q_i, k_i, w_i, q_b, k_b, v_b = split_cols(x @ w_in, EVEN_WIDTHS)
    o_a = dsa_attention(q_a, c_kv, q_i, k_i, w_i, ckv_gain, w_uk, w_uv, rel_table[:, :A_HEADS])
    o_b = banded_attention(q_b.reshape(B, S, B_HEADS, HEAD_DIM),
                           k_b.reshape(B, S, B_KV_HEADS, HEAD_DIM),
                           v_b.reshape(B, S, B_KV_HEADS, HEAD_DIM),
                           rel_table[:, A_HEADS:A_HEADS + B_HEADS], B_WINDOW, sinks)
    o = jnp.concatenate([o_a.reshape(B, S, -1), o_b.reshape(B, S, -1)], axis=-1)
    return o @ w_out


def odd_mixer(x, w_in, pe_k, w1_k, w2_k, pe_v, w1_v, w2_v, w_out, rel_table):
    B, S, _ = x.shape
    q, kc, vc, ks, vs, kw, vw, gl = split_cols(x @ w_in, ODD_WIDTHS)
    r = lambda a, h: a.reshape(B, S, h, HEAD_DIM)
    o = nsa_attention(r(q, C_HEADS), r(kc, C_KV_HEADS), r(vc, C_KV_HEADS), r(ks, C_KV_HEADS),
                      r(vs, C_KV_HEADS), r(kw, C_KV_HEADS), r(vw, C_KV_HEADS), gl,
                      pe_k, w1_k, w2_k, pe_v, w1_v, w2_v, rel_table[:, :C_HEADS])
    return o.reshape(B, S, C_HEADS * HEAD_DIM) @ w_out


def cross_attention(x, mem, w_q, w_k, w_v, w_o):
    B, S, _ = x.shape
    M = mem.shape[1]
    q = (x @ w_q).reshape(B, S, X_HEADS, X_HEAD_DIM)
    k = (mem @ w_k).reshape(B, M, X_HEADS, X_HEAD_DIM)
    v = (mem @ w_v).reshape(B, M, X_HEADS, X_HEAD_DIM)
    lg = jnp.einsum('bshd,bmhd->bhsm', q, k).astype(jnp.float32) * X_HEAD_DIM ** -0.5
    p = jax.nn.softmax(lg, axis=-1).astype(v.dtype)
    o = jnp.einsum('bhsm,bmhd->bshd', p, v).reshape(B, S, X_HEADS * X_HEAD_DIM)
    return o @ w_o


def dense_swiglu(x, w_gate, w_up, w_down):
    return (jax.nn.silu(x @ w_gate) * (x @ w_up)) @ w_down


def moe_swiglu(x, w_router, w_gate, w_up, w_down):
    logits = (x @ w_router).astype(jnp.float32)
    top_val, top_idx = lax.top_k(logits, TOP_K)
    top_w = jax.nn.softmax(top_val, axis=-1)
    gate = jnp.einsum('bsk,bske->bse', top_w,
                      jax.nn.one_hot(top_idx, N_EXPERTS, dtype=jnp.float32)).astype(x.dtype)
    y = jnp.zeros_like(x)
    for e in range(N_EXPERTS):
        h = jax.nn.silu(x @ w_gate[e]) * (x @ w_up[e])
        y = y + gate[..., e:e + 1] * (h @ w_down[e])
    return y


def setup_inputs(seed: int = 0) -> dict:
    key = jax.random.key(seed)
    ks = iter(jax.random.split(key, 32))

    def nrm(shape, scale):
        return jax.random.normal(next(ks), shape, jnp.float32) * scale

    D = D_MODEL
    XW = X_HEADS * X_HEAD_DIM
    CW = CMP_BLOCK * HEAD_DIM
    return {
        'x': nrm((BATCH, SEQ, D), 1.0),
        'mem': nrm((BATCH, MEM_LEN, D), 1.0),
        'rel_table': nrm((REL_BUCKETS, N_SLOTS), 0.2),
        'ev_w_in': nrm((N_EVEN, D, EVEN_COLS), D ** -0.5),
        'ev_ckv_gain': 1.0 + nrm((N_EVEN, A_LATENT), 0.02),
        'ev_w_uk': nrm((N_EVEN, A_HEADS, HEAD_DIM, A_LATENT), HEAD_DIM ** -0.5),
        'ev_w_uv': nrm((N_EVEN, A_HEADS, A_LATENT, HEAD_DIM), A_LATENT ** -0.5),
        'ev_sinks': nrm((N_EVEN, B_HEADS), 0.5),
        'ev_w_out': nrm((N_EVEN, N_SLOTS * HEAD_DIM, D), BETA * (N_SLOTS * HEAD_DIM) ** -0.5),
        'od_w_in': nrm((N_ODD, D, ODD_COLS), D ** -0.5),
        'od_pe_k': nrm((N_ODD, CMP_BLOCK, HEAD_DIM), 0.1),
        'od_w1_k': nrm((N_ODD, CW, CMP_HIDDEN), CW ** -0.5),
        'od_w2_k': nrm((N_ODD, CMP_HIDDEN, HEAD_DIM), CMP_HIDDEN ** -0.5),
        'od_pe_v': nrm((N_ODD, CMP_BLOCK, HEAD_DIM), 0.1),
        'od_w1_v': nrm((N_ODD, CW, CMP_HIDDEN), CW ** -0.5),
        'od_w2_v': nrm((N_ODD, CMP_HIDDEN, HEAD_DIM), CMP_HIDDEN ** -0.5),
        'od_w_out': nrm((N_ODD, C_HEADS * HEAD_DIM, D), BETA * (C_HEADS * HEAD_DIM) ** -0.5),
        'xa_w_q': nrm((DEPTH, D, XW), D ** -0.5),
        'xa_w_k': nrm((DEPTH, D, XW), D ** -0.5),
        'xa_w_v': nrm((DEPTH, D, XW), D ** -0.5),
        'xa_w_o': nrm((DEPTH, XW, D), BETA * XW ** -0.5),
        'ff_w_gate': nrm((N_EVEN, D, D_FF), D ** -0.5),
        'ff_w_up': nrm((N_EVEN, D, D_FF), D ** -0.5),
        'ff_w_down': nrm((N_EVEN, D_FF, D), BETA * D_FF ** -0.5),
        'moe_w_router': nrm((N_ODD, D, N_EXPERTS), D ** -0.5),
        'moe_w_gate': nrm((N_ODD, N_EXPERTS, D, D_FF_EXPERT), D ** -0.5),
        'moe_w_up': nrm((N_ODD, N_EXPERTS, D, D_FF_EXPERT), D ** -0.5),
        'moe_w_down': nrm((N_ODD, N_EXPERTS, D_FF_EXPERT, D), BETA * D_FF_EXPERT ** -0.5),
        'ln_g': 1.0 + nrm((DEPTH, 3, D), 0.02),
        'ln_b': nrm((DEPTH, 3, D), 0.02),
    }


def reference(x, mem, rel_table, ev_w_in, ev_ckv_gain, ev_w_uk, ev_w_uv, ev_sinks, ev_w_out,
              od_w_in, od_pe_k, od_w1_k, od_w2_k, od_pe_v, od_w1_v, od_w2_v, od_w_out,
              xa_w_q, xa_w_k, xa_w_v, xa_w_o, ff_w_gate, ff_w_up, ff_w_down,
              moe_w_router, moe_w_gate, moe_w_up, moe_w_down, ln_g, ln_b):
    h = x
    for i in range(DEPTH):
        j = i // 2
        if i % 2 == 0:
            mix = even_mixer(h, ev_w_in[j], ev_ckv_gain[j], ev_w_uk[j], ev_w_uv[j], ev_sinks[j],
                             ev_w_out[j], rel_table)
        else:
            mix = odd_mixer(h, od_w_in[j], od_pe_k[j], od_w1_k[j], od_w2_k[j], od_pe_v[j],
                            od_w1_v[j], od_w2_v[j], od_w_out[j], rel_table)
        h = layer_norm(ALPHA * h + mix, ln_g[i, 0], ln_b[i, 0])
        xa = cross_attention(h, mem, xa_w_q[i], xa_w_k[i], xa_w_v[i], xa_w_o[i])
        h = layer_norm(ALPHA * h + xa, ln_g[i, 1], ln_b[i, 1])
        if i % 2 == 0:
            ff = dense_swiglu(h, ff_w_gate[j], ff_w_up[j], ff_w_down[j])
        else:
            ff = moe_swiglu(h, moe_w_router[j], moe_w_gate[j], moe_w_up[j], moe_w_down[j])
        h = layer_norm(ALPHA * h + ff, ln_g[i, 2], ln_b[i, 2])
    return h
```

```python
import math
from contextlib import ExitStack

import numpy as np
import concourse.bass as bass
import concourse.mybir as mybir
from concourse.bass_utils import run_bass_kernel_spmd

F32 = mybir.dt.float32
BF16 = mybir.dt.bfloat16
AF = mybir.ActivationFunctionType
ALU = mybir.AluOpType
AX = mybir.AxisListType

NCORES = 8
NB = 2
S = 2048
NT = 16
D = 1024
KC = 8
ALPHA = 4 ** 0.25
LN_EPS = 1e-5
D_FF = 2816
N_EXP = 8
D_FFE = 3584
NEG = -30000.0
import os
STOP = int(os.environ.get('KSTOP', '0'))
KVAR = int(os.environ.get('KVAR', '0'))
NTM = 256 if (KVAR & 32) else 264


class Sem:
    def __init__(self, nc, name):
        self.h = nc.alloc_semaphore(name)
        self.count = 0


class Sched:
    def __init__(self, nc):
        self.nc = nc
        self.eng = {'pe': nc.tensor, 'act': nc.scalar, 'dve': nc.vector, 'pool': nc.gpsimd, 'sp': nc.sync}
        self.sem = {k: Sem(nc, 's_' + k) for k in self.eng}
        self.waited = {k: {} for k in self.eng}
        self.lastw = {}
        self.readers = {}
        self.dsem = {}
        self.ninst = 0

    def _wait(self, e, deps):
        w = self.waited[e]
        best = {}
        for (s, v) in deps:
            if w.get(s, 0) < v and best.get(s, 0) < v:
                best[s] = v
        for s, v in best.items():
            self.eng[e].wait_ge(s.h, v)
            w[s] = v

    def _deps(self, e, reads, writes):
        deps = []
        mine = self.sem.get(e)
        for k in reads:
            t = self.lastw.get(k)
            if t is not None:
                deps.append(t)
            if k.startswith('ps') or k == 'pt':
                deps.extend(r for r in self.readers.get(k, ()) if r[0] is not mine)
        for k in writes:
            t = self.lastw.get(k)
            if t is not None:
                deps.append(t)
            deps.extend(self.readers.get(k, ()))
        if e == 'pe':
            pes = self.sem['pe']
            deps = [d for d in deps if d[0] is not pes]
        return deps

    def _commit(self, tok, reads, writes):
        for k in writes:
            self.lastw[k] = tok
            self.readers[k] = []
        for k in reads:
            if k in writes:
                continue
            r = self.readers.setdefault(k, [])
            r[:] = [t for t in r if t[0] is not tok[0]]
            r.append(tok)

    def op(self, e, fn, reads=(), writes=()):
        self._wait(e, self._deps(e, reads, writes))
        inst = fn(self.eng[e])
        s = self.sem[e]
        s.count += 1
        inst.then_inc(s.h, 1)
        self._commit((s, s.count), reads, writes)
        self.ninst += 1

    def dma(self, e, out, in_, reads=(), writes=(), slot=None):
        self._wait(e, self._deps(e, reads, writes))
        if slot is None:
            slot = writes[0]
        if slot not in self.dsem:
            self.dsem[slot] = Sem(self.nc, 'd%d' % len(self.dsem))
        s = self.dsem[slot]
        inst = self.eng[e].dma_start(out=out, in_=in_)
        s.count += 16
        inst.then_inc(s.h, 16)
        self._commit((s, s.count), reads, writes)
        self.ninst += 1

    def barrier(self):
        allsem = list(self.sem.values()) + list(self.dsem.values())
        for e in self.eng:
            self._wait(e, [(s, s.count) for s in allsem if s.count > 0])
        self.lastw.clear()
        self.readers.clear()


def _rel_bucket_np(dist):
    n = np.maximum(dist, 0)
    nf = np.maximum(n, 1).astype(np.float32)
    log_b = 16 + (np.log(nf / np.float32(16)) / np.float32(math.log(8.0)) * np.float32(16)).astype(np.int32)
    return np.where(n < 16, n, np.minimum(log_b, 31)).astype(np.int64)


class Prog:
    def __init__(self, phases, dbg=False):
        self.phases = phases
        self.dbg = dbg
        nc = bass.Bass("TRN2", target_bir_lowering=False)
        self.nc = nc
        self.S = Sched(nc)
        self.t = {}
        self.declare_io()
        self.build()

    def din(self, name, shape):
        self.t[name] = self.nc.dram_tensor(name, list(shape), F32, kind="ExternalInput").ap()
        return self.t[name]

    def declare_io(self):
        nc = self.nc
        self.din('x', [NB, S, D])
        self.din('mem', [NB, 256, D])
        self.din('ev_w_in', [D, 1992])
        self.din('ev_ckv_gain', [1, 128])
        self.din('ev_w_uk', [8, 64, 128])
        self.din('ev_w_uv', [8, 128, 64])
        self.din('ev_sinks', [1, 8])
        self.din('ev_w_out', [D, D])
        self.din('od_w_in', [D, 1840])
        self.din('od_pe_k', [32, 64])
        self.din('od_w1_k', [2048, 128])
        self.din('od_w2_k', [128, 64])
        self.din('od_pe_v', [32, 64])
        self.din('od_w1_v', [2048, 128])
        self.din('od_w2_v', [128, 64])
        self.din('od_w_out', [D, D])
        self.din('xa_w_q', [2, D, 512])
        self.din('xa_w_k', [2, D, 512])
        self.din('xa_w_v', [2, D, 512])
        self.din('xa_w_o', [2, 512, D])
        self.din('ff_w_gate', [D, D_FF])
        self.din('ff_w_up', [D, D_FF])
        self.din('ff_w_down', [D_FF, D])
        self.din('moe_w_router', [D, N_EXP])
        self.din('moe_w_gate', [N_EXP, D, D_FFE])
        self.din('moe_w_up', [N_EXP, D, D_FFE])
        self.din('moe_w_down', [N_EXP, D_FFE, D])
        self.din('ln_g', [2, 3, D])
        self.din('ln_b', [2, 3, D])
        self.din('bt_tab', [16, 128, 256])
        self.din('cfar', [1, 16])
        self.din('cb_tab', [16, 4, 127, 512])
        self.din('ovl', [127, 33])
        self.din('eblk', [32, S])
        self.din('gsel', [48, 24, 128])
        self.din('seltab', [3, 16, 128, 64])
        self.out = nc.dram_tensor("out", [NB, S, D], F32, kind="ExternalOutput").ap()
        self.hscr = nc.dram_tensor("hscr", [NB, S, D], F32, kind="Internal").ap()

    def sb(self, st, name, shape, dt):
        self.uid = getattr(self, 'uid', 0) + 1
        return st.enter_context(self.nc.sbuf_tensor('%s_u%d' % (name, self.uid), list(shape), dt))

    def bcast_rows(self, ap_row, n):
        return bass.AP(ap_row.tensor, ap_row.offset, [[0, 128], [1, n]])

    def build(self):
        nc, S_ = self.nc, self.S
        with ExitStack() as gst:
            self.ident = self.sb(gst, 'ident', [128, 128], BF16)
            self.ones = self.sb(gst, 'ones', [128, 128], BF16)
            self.hT = self.sb(gst, 'hT', [128, KC, S], BF16)
            self.ps = [gst.enter_context(nc.psum_tensor('ps%d' % i, [128, 512], F32)) for i in range(7)]
            self.pt = gst.enter_context(nc.psum_tensor('pt', [128, 1024], BF16))
            S_.op('pool', lambda e: e.memset(self.ident[:], 1.0), writes=['ident'])
            S_.op('pool', lambda e: e.affine_select(out=self.ident[:], in_=self.ident[:], pattern=[[-1, 128]],
                                                      compare_op=ALU.is_equal, fill=0.0, base=0, channel_multiplier=1),
                  reads=['ident'], writes=['ident'])
            S_.op('pool', lambda e: e.memset(self.ones[:], 1.0), writes=['ones'])
            self.eps_t = self.sb(gst, 'eps_t', [128, 2], F32)
            S_.op('pool', lambda e: e.memset(self.eps_t[:], LN_EPS), writes=['eps_t'])
            S_.barrier()
            for b in range(NB):
                cur = ('x', b)
                for ph in self.phases:
                    if ph == 'x0':
                        self.phase_x0(b)
                    elif ph.startswith('xattn'):
                        i = int(ph[-1])
                        last = (ph == self.phases[-1])
                        self.phase_xattn(b, i, cur, ('out', b) if last else ('hscr', b))
                        cur = ('hscr', b)
                    elif ph == 'ffn':
                        last = (ph == self.phases[-1])
                        self.phase_ffn(b, cur, ('out', b) if last else ('hscr', b), moe=False)
                        cur = ('hscr', b)
                    elif ph == 'moe':
                        last = (ph == self.phases[-1])
                        self.phase_ffn(b, cur, ('out', b) if last else ('hscr', b), moe=True)
                        cur = ('hscr', b)
                    elif ph == 'mix0':
                        last = (ph == self.phases[-1])
                        self.phase_mix0(b, cur, ('out', b) if last else ('hscr', b))
                        cur = ('hscr', b)
                    elif ph == 'mix1':
                        last = (ph == self.phases[-1])
                        self.phase_mix1(b, cur, ('out', b) if last else ('hscr', b))
                        cur = ('hscr', b)
                    else:
                        raise ValueError(ph)
                    S_.barrier()
            S_.barrier()

    def dram_tile(self, loc, t):
        kind, b = loc
        base = {'x': self.t['x'], 'hscr': self.hscr, 'out': self.out}[kind]
        return base[b, t * 128:(t + 1) * 128, :]

    def to_hT(self, src_bf, src_key, t, eng='dve'):
        S_ = self.S
        for kc in range(KC):
            S_.op('pe', lambda e: e.transpose(self.pt[:, kc * 128:(kc + 1) * 128], src_bf[:, kc * 128:(kc + 1) * 128],
                                              self.ident[:]),
                  reads=[src_key, 'ident'], writes=['pt'])
        dst = self.hT[:, :, t * 128:(t + 1) * 128]
        src = self.pt[:].rearrange("p (k n) -> p k n", k=KC)
        hk = 'hT%d' % (t // 4)
        if eng == 'dve':
            S_.op('dve', lambda e: e.tensor_copy(out=dst, in_=src), reads=['pt'], writes=[hk])
        else:
            S_.op('act', lambda e: e.activation(out=dst, in_=src, func=AF.Copy), reads=['pt'], writes=[hk])


    def run_blocks(self, blocks):
        n = len(blocks)
        for i in range(n + 1):
            if i < n:
                sj = 2 + (i % 2)
                blocks[i]['qk'](self.ps[sj], 'ps%d' % sj, i % 2)
            if i >= 1:
                b_ = blocks[i - 1]
                b_['pv']((i - 1) % 2)
                if b_.get('post'):
                    b_['post']()

    def phase_x0(self, b):
        S_ = self.S
        with ExitStack() as st:
            xb = [self.sb(st, 'x0b%d' % i, [128, D], BF16) for i in range(2)]
            for t in range(NT):
                k = 'x0b%d' % (t % 2)
                S_.dma('pool', xb[t % 2][:], self.t['x'][b, t * 128:(t + 1) * 128, :], writes=[k])
                self.to_hT(xb[t % 2], k, t, eng='dve' if t % 2 == 0 else 'act')
            self.S.barrier()

    def ln_setup(self, st, i, k, alias=None):
        S_ = self.S
        self.ln_g = self.sb(st, 'ln_g', [128, D], F32)
        self.ln_b = self.sb(st, 'ln_b', [128, D], F32)
        S_.dma('sp', self.ln_g[:], self.bcast_rows(self.t['ln_g'][i, k:k + 1, :], D), writes=['ln_g'])
        S_.dma('sp', self.ln_b[:], self.bcast_rows(self.t['ln_b'][i, k:k + 1, :], D), writes=['ln_b'])
        if alias is None:
            self.ln_res = [self.sb(st, 'ln_res0', [128, D], F32)] * 2
            self.ln_t = [self.sb(st, 'ln_t0', [128, D], F32)] * 2
            self.ln_keys = ('ln_res0', 'ln_t0')
        else:
            buf, key = alias
            self.ln_res = [buf[:, 0:D]] * 2
            self.ln_t = [buf[:, D:2 * D]] * 2
            self.ln_keys = (key, key)
        self.ln_hb = [self.sb(st, 'ln_hb0', [128, D], BF16)] * 2
        self.ln_st = [self.sb(st, 'ln_st%d' % j, [128, 16], F32) for j in range(2)]
        self.ln_cnt = 0

    def ln_tile(self, srcs, src_keys, t, res_loc, out_loc, want_hT=True):
        S_ = self.S
        j = self.ln_cnt % 2
        self.ln_cnt += 1
        res, tt, hb, stt = self.ln_res[j], self.ln_t[j], self.ln_hb[j], self.ln_st[j]
        kres, kt, khb, kst = self.ln_keys[0], self.ln_keys[1], 'ln_hb0', 'ln_st%d' % j
        S_.dma('sp', res[:], self.dram_tile(res_loc, t), reads=['dram_%s_%d' % (res_loc[0], t)], writes=[kres])
        for hf in range(2):
            sl = slice(hf * 512, (hf + 1) * 512)
            S_.op('dve', lambda e: e.scalar_tensor_tensor(out=tt[:, sl], in0=res[:, sl], scalar=ALPHA, in1=srcs[hf],
                                                          op0=ALU.mult, op1=ALU.add),
                  reads=[kres, src_keys[hf]], writes=[kt])
        for hf in range(2):
            sl = slice(hf * 512, (hf + 1) * 512)
            S_.op('dve', lambda e: e.bn_stats(out=stt[:, hf * 6:(hf + 1) * 6], in_=tt[:, sl]), reads=[kt], writes=[kst])
        S_.op('dve', lambda e: e.bn_aggr(out=stt[:, 12:14], in_=stt[:, 0:12]),
              reads=[kst], writes=[kst])
        S_.op('act', lambda e: e.activation(out=stt[:, 14:15], in_=stt[:, 13:14], func=AF.Ln, bias=self.eps_t[:, 0:1], scale=1.0),
              reads=[kst, 'eps_t'], writes=[kst])
        S_.op('act', lambda e: e.activation(out=stt[:, 14:15], in_=stt[:, 14:15], func=AF.Exp, scale=-0.5), reads=[kst], writes=[kst])
        S_.op('dve', lambda e: e.scalar_tensor_tensor(out=stt[:, 15:16], in0=stt[:, 12:13], scalar=-1.0, in1=stt[:, 14:15],
                                                      op0=ALU.mult, op1=ALU.mult), reads=[kst], writes=[kst])
        S_.op('act', lambda e: e.activation(out=tt[:], in_=tt[:], func=AF.Identity, bias=stt[:, 15:16], scale=stt[:, 14:15]),
              reads=[kt, kst], writes=[kt])
        S_.op('pool', lambda e: e.tensor_tensor(out=tt[:], in0=tt[:], in1=self.ln_g[:], op=ALU.mult),
              reads=[kt, 'ln_g'], writes=[kt])
        S_.op('pool', lambda e: e.tensor_tensor(out=tt[:], in0=tt[:], in1=self.ln_b[:], op=ALU.add),
              reads=[kt, 'ln_b'], writes=[kt])
        S_.dma('sp', self.dram_tile(out_loc, t), tt[:], reads=[kt], writes=['dram_%s_%d' % (out_loc[0], t)],
               slot='st_%d' % j)
        if want_hT:
            S_.op('act', lambda e: e.activation(out=hb[:], in_=tt[:], func=AF.Copy), reads=[kt], writes=[khb])
            self.to_hT(hb, khb, t, eng='dve')

    def load_w(self, dst, src, key):
        self.S.dma('pool', dst, src, writes=[key])

    def phase_xattn(self, b, i, res_loc, out_loc):
        S_, ps = self.S, self.ps
        sc = 128 ** -0.5
        with ExitStack() as st:
            wq = self.sb(st, 'xwq', [128, KC, 512], BF16)
            wk = self.sb(st, 'xwk', [128, KC, 512], BF16)
            wv = self.sb(st, 'xwv', [128, KC, 512], BF16)
            wo = self.sb(st, 'xwo', [128, 4, D], BF16)
            memb = self.sb(st, 'memb', [128, 2, D], BF16)
            memT = self.sb(st, 'memT', [128, KC, 256], BF16)
            kxT = self.sb(st, 'kxT', [128, 4, 256], BF16)
            vx = self.sb(st, 'vx', [128, 2, 512], BF16)
            qxT = self.sb(st, 'qxT', [128, 4, 512], BF16)
            xoT = self.sb(st, 'xoT', [128, 4, 512], BF16)
            pT = [self.sb(st, 'xpT%d' % j, [128, 512], BF16) for j in range(2)]
            rd = self.sb(st, 'xrd', [128, 512], F32)
            self.ln_setup(st, i, 1)
            self.load_w(wk[:], self.t['xa_w_k'][i].rearrange("(k p) n -> p k n", p=128), 'xwk')
            self.load_w(wv[:], self.t['xa_w_v'][i].rearrange("(k p) n -> p k n", p=128), 'xwv')
            self.load_w(memb[:], self.t['mem'][b].rearrange("(t p) d -> p t d", p=128), 'memb')
            self.load_w(wq[:], self.t['xa_w_q'][i].rearrange("(k p) n -> p k n", p=128), 'xwq')
            self.load_w(wo[:], self.t['xa_w_o'][i].rearrange("(k p) n -> p k n", p=128), 'xwo')
            for mt in range(2):
                for kc in range(KC):
                    S_.op('pe', lambda e: e.transpose(self.pt[:, kc * 128:(kc + 1) * 128],
                                                      memb[:, mt, kc * 128:(kc + 1) * 128], self.ident[:]),
                          reads=['memb', 'ident'], writes=['pt'])
                S_.op('dve', lambda e: e.tensor_copy(out=memT[:, :, mt * 128:(mt + 1) * 128],
                                                     in_=self.pt[:].rearrange("p (k n) -> p k n", k=KC)),
                      reads=['pt'], writes=['memT'])
            for h in range(4):
                for kc in range(KC):
                    S_.op('pe', lambda e: e.matmul(ps[0][:, 0:256], wk[:, kc, h * 128:(h + 1) * 128], memT[:, kc, :],
                                                   start=(kc == 0), stop=(kc == KC - 1)),
                          reads=['xwk', 'memT'], writes=['ps0'])
                S_.op('act', lambda e: e.activation(out=kxT[:, h, :], in_=ps[0][:, 0:256], func=AF.Copy),
                      reads=['ps0'], writes=['kxT'])
            for mt in range(2):
                for kc in range(KC):
                    S_.op('pe', lambda e: e.matmul(ps[1][:], memT[:, kc, mt * 128:(mt + 1) * 128], wv[:, kc, :],
                                                   start=(kc == 0), stop=(kc == KC - 1)),
                          reads=['xwv', 'memT'], writes=['ps1'])
                S_.op('dve', lambda e: e.tensor_copy(out=vx[:, mt, :], in_=ps[1][:]), reads=['ps1'], writes=['vx'])
            for c in range(4):
                hk = 'hT%d' % c
                for h in range(4):
                    for kc in range(KC):
                        S_.op('pe', lambda e: e.matmul(ps[h % 2][:], wq[:, kc, h * 128:(h + 1) * 128],
                                                       self.hT[:, kc, c * 512:(c + 1) * 512],
                                                       start=(kc == 0), stop=(kc == KC - 1)),
                              reads=['xwq', hk], writes=['ps%d' % (h % 2)])
                    if h % 2 == 0:
                        S_.op('act', lambda e: e.activation(out=qxT[:, h, :], in_=ps[h % 2][:], func=AF.Copy),
                              reads=['ps%d' % (h % 2)], writes=['qxT%d' % h])
                    else:
                        S_.op('dve', lambda e: e.tensor_copy(out=qxT[:, h, :], in_=ps[h % 2][:]),
                              reads=['ps%d' % (h % 2)], writes=['qxT%d' % h])
                blocks = []
                for h in range(4):
                    for mt in range(2):
                        def qk(sT, sk, pj, h=h, mt=mt):
                            S_.op('pe', lambda e: e.matmul(sT[:], kxT[:, h, mt * 128:(mt + 1) * 128], qxT[:, h, :],
                                                           start=True, stop=True), reads=['kxT', 'qxT%d' % h], writes=[sk])
                            S_.op('act', lambda e: e.activation(out=pT[pj][:], in_=sT[:], func=AF.Exp, scale=sc),
                                  reads=[sk], writes=['xpT%d' % pj])

                        def pv(pj, h=h, mt=mt):
                            S_.op('pe', lambda e: e.matmul(ps[4][:], vx[:, mt, h * 128:(h + 1) * 128], pT[pj][:],
                                                           start=(mt == 0), stop=(mt == 1)), reads=['vx', 'xpT%d' % pj], writes=['ps4'])
                            S_.op('pe', lambda e: e.matmul(ps[5][:], self.ones[:], pT[pj][:],
                                                           start=(mt == 0), stop=(mt == 1)), reads=['ones', 'xpT%d' % pj], writes=['ps5'])

                        def post(h=h):
                            S_.op('act', lambda e: e.activation(out=rd[:], in_=ps[5][:], func=AF.Ln), reads=['ps5'], writes=['xrd'])
                            S_.op('act', lambda e: e.activation(out=rd[:], in_=rd[:], func=AF.Exp, scale=-1.0), reads=['xrd'], writes=['xrd'])
                            S_.op('dve', lambda e: e.tensor_tensor(out=xoT[:, h, :], in0=rd[:], in1=ps[4][:], op=ALU.mult),
                                  reads=['xrd', 'ps4'], writes=['xoT'])
                        blocks.append({'qk': qk, 'pv': pv, 'post': post if mt == 1 else None})
                self.run_blocks(blocks)
                for tt in range(4):
                    for hf in range(2):
                        for h in range(4):
                            S_.op('pe', lambda e: e.matmul(ps[hf][:], xoT[:, h, tt * 128:(tt + 1) * 128],
                                                           wo[:, h, hf * 512:(hf + 1) * 512],
                                                           start=(h == 0), stop=(h == 3)),
                                  reads=['xoT', 'xwo'], writes=['ps%d' % hf])
                    self.ln_tile([ps[0][:], ps[1][:]], ['ps0', 'ps1'], c * 4 + tt, res_loc, out_loc)
            S_.barrier()

    def phase_mix0(self, b, res_loc, out_loc):
        S_, ps, nc = self.S, self.ps, self.nc
        W = self.t['ev_w_in']
        NIT = 14
        with ExitStack() as st:
            wqa = self.sb(st, 'wqa', [128, KC, 512], BF16)
            wqi = self.sb(st, 'wqi', [128, KC, 512], BF16)
            wqb = self.sb(st, 'wqb', [128, KC, 512], BF16)
            wuk = self.sb(st, 'wuk', [128, 4, 128], BF16)
            wout = self.sb(st, 'wout', [128, KC, D], BF16)
            cT = self.sb(st, 'cT', [128, S], BF16)
            vA = self.sb(st, 'vA', [128, NT, 8, 128], BF16)
            kiT2 = self.sb(st, 'kiT2', [128, S], BF16)
            kbT = self.sb(st, 'kbT', [128, S], BF16)
            vb = self.sb(st, 'vb', [128, NT, 2, 128], BF16)
            absw = self.sb(st, 'absw', [128, NT, 8], F32)
            sgnw = self.sb(st, 'sgnw', [128, NT, 8], F32)
            TB = self.sb(st, 'TB', [128, 16, 256], BF16)
            cfar = self.sb(st, 'cfar', [128, 16], F32)
            esink = self.sb(st, 'esink', [128, 8], F32)
            tri = self.sb(st, 'tri', [128, 128], F32)
            pw = self.sb(st, 'pw', [128, NIT], F32)
            self.load_w(wqa[:], W[:, 0:512].rearrange("(k p) n -> p k n", p=128), 'wqa')
            self.load_w(wqi[:], W[:, 640:1152].rearrange("(k p) n -> p k n", p=128), 'wqi')
            for hf in range(2):
                for j in range(4):
                    c0 = 1224 + (hf * 4 + j) * 64
                    self.load_w(wqb[:, :, j * 128 + hf * 64:j * 128 + (hf + 1) * 64],
                                W[:, c0:c0 + 64].rearrange("(k p) n -> p k n", p=128), 'wqb')
            for hh in range(2):
                self.load_w(wuk[hh * 64:(hh + 1) * 64, :, :],
                            self.t['ev_w_uk'].rearrange("(j f) d c -> f d j c", f=2)[hh], 'wuk')
            self.load_w(wout[:], self.t['ev_w_out'].rearrange("(k p) n -> p k n", p=128), 'wout')
            S_.dma('sp', cfar[:], self.bcast_rows(self.t['cfar'][0:1, :], 16), writes=['cfar'])
            S_.dma('sp', esink[:], self.bcast_rows(self.t['ev_sinks'][0:1, :], 8), writes=['esink'])
            S_.op('act', lambda e: e.activation(out=esink[:], in_=esink[:], func=AF.Exp), reads=['esink'], writes=['esink'])
            esink2 = self.sb(st, 'esink2', [128, 4], F32)
            ev = esink[:].rearrange("p (m f) -> p m f", f=2)
            S_.op('dve', lambda e: e.tensor_copy(out=esink2[0:64, :], in_=ev[0:64, :, 0]), reads=['esink'], writes=['esink2'])
            S_.op('dve', lambda e: e.tensor_copy(out=esink2[64:128, :], in_=ev[64:128, :, 1]), reads=['esink'], writes=['esink2'])
            S_.op('pool', lambda e: e.memset(tri[:], 0.0), writes=['tri'])
            S_.op('pool', lambda e: e.memset(vb[:], 1.0), writes=['vb'])
            S_.op('pool', lambda e: e.memset(vA[:], 1.0), writes=['vA'])
            S_.op('pool', lambda e: e.affine_select(out=tri[:], in_=tri[:], pattern=[[-1, 128]], compare_op=ALU.is_ge,
                                                      fill=-1e30, base=0, channel_multiplier=1), reads=['tri'], writes=['tri'])
            for i in range(NIT):
                S_.op('pool', lambda e: e.memset(pw[:, i:i + 1], 2.0 ** (-i)), writes=['pw'])
            with ExitStack() as st2:
                bt = [self.sb(st2, 'bt%d' % i, [128, 256], F32) for i in range(2)]
                swam = self.sb(st2, 'swam', [128, 256], F32)
                S_.op('pool', lambda e: e.memset(swam[:], 0.0), writes=['swam'])
                S_.op('pool', lambda e: e.affine_select(out=swam[:], in_=swam[:], pattern=[[1, 256]], compare_op=ALU.is_ge,
                                                          fill=NEG, base=0, channel_multiplier=-1), reads=['swam'], writes=['swam'])
                S_.op('pool', lambda e: e.affine_select(out=swam[:], in_=swam[:], pattern=[[-1, 256]], compare_op=ALU.is_ge,
                                                          fill=NEG, base=127, channel_multiplier=1), reads=['swam'], writes=['swam'])
                for h in range(16):
                    k = 'bt%d' % (h % 2)
                    S_.dma('sp', bt[h % 2][:], self.t['bt_tab'][h], writes=[k])
                    if h < 8:
                        S_.op('dve', lambda e: e.tensor_scalar(out=TB[:, h, :], in0=bt[h % 2][:], scalar1=cfar[:, h:h + 1],
                                                               scalar2=8.0, op0=ALU.subtract, op1=ALU.mult),
                              reads=[k, 'cfar'], writes=['TB'])
                    else:
                        S_.op('dve', lambda e: e.scalar_tensor_tensor(out=TB[:, h, :], in0=bt[h % 2][:], scalar=8.0, in1=swam[:],
                                                                      op0=ALU.mult, op1=ALU.add),
                              reads=[k, 'swam'], writes=['TB'])
                S_.barrier()
            if STOP == 1:
                return
            with ExitStack() as st2:
                wtm = self.sb(st2, 'wtm', [128, KC, 288], BF16)
                wki2 = self.sb(st2, 'wki2', [128, KC, 128], BF16)
                wkb = self.sb(st2, 'wkb', [128, KC, 128], BF16)
                wuv = self.sb(st2, 'wuv', [128, 8, 64], BF16)
                gain = self.sb(st2, 'gain', [128, 128], F32)
                cb = [self.sb(st2, 'cb%d' % i, [128, 128], BF16) for i in range(2)]
                sq = self.sb(st2, 'sq', [128, 128], F32)
                ss = [self.sb(st2, 'ss%d' % i, [128, 4], F32) for i in range(2)]
                r3 = lambda a: a.rearrange("(k p) n -> p k n", p=128)
                self.load_w(wtm[:, :, 0:128], r3(W[:, 512:640]), 'wtm')
                self.load_w(wtm[:, :, 128:256], r3(W[:, 1864:1992]), 'wtm')
                if not (KVAR & 1):
                    self.load_w(wtm[:, :, 256:264], r3(W[:, 1216:1224]), 'wtm')
                else:
                    S_.op('pool', lambda e: e.memset(wtm[:, :, 256:264], 0.5), writes=['wtm'])
                self.load_w(wki2[:, :, 0:64], r3(W[:, 1152:1216]), 'wki2')
                self.load_w(wki2[:, :, 64:128], r3(W[:, 1152:1216]), 'wki2')
                self.load_w(wkb[:], r3(W[:, 1736:1864]), 'wkb')
                self.load_w(wuv[:], self.t['ev_w_uv'].rearrange("h c d -> c h d"), 'wuv')
                S_.dma('sp', gain[:], self.bcast_rows(self.t['ev_ckv_gain'][0:1, :], 128), writes=['gain'])
                if STOP == 21:
                    S_.barrier()
                    return
                for t in range(NT if STOP != 22 else 1):
                    hk = 'hT%d' % (t // 4)
                    i2 = t % 2
                    for kc in range(KC):
                        S_.op('pe', lambda e: e.matmul(ps[0][:, 0:NTM], self.hT[:, kc, t * 128:(t + 1) * 128], wtm[:, kc, 0:NTM],
                                                       start=(kc == 0), stop=(kc == KC - 1)), reads=[hk, 'wtm'], writes=['ps0'])
                    if not (KVAR & 4):
                        S_.op('act', lambda e: e.activation(out=sq[:], in_=ps[0][:, 0:128], func=AF.Square,
                                                            accum_out=ss[i2][:, 0:1]), reads=['ps0'], writes=['sq', 'ss%d' % i2])
                    if not (KVAR & 4):
                        S_.op('act', lambda e: e.activation(out=ss[i2][:, 1:2], in_=ss[i2][:, 0:1], func=AF.Ln, bias=self.eps_t[:, 0:1],
                                                            scale=1.0 / 128), reads=['ss%d' % i2, 'eps_t'], writes=['ss%d' % i2])
                    if not (KVAR & 4):
                        S_.op('act', lambda e: e.activation(out=ss[i2][:, 2:3], in_=ss[i2][:, 1:2], func=AF.Exp, scale=-0.5),
                              reads=['ss%d' % i2], writes=['ss%d' % i2])
                    if not (KVAR & 64):
                        S_.op('dve', lambda e: e.scalar_tensor_tensor(out=cb[i2][:], in0=ps[0][:, 0:128], scalar=ss[i2][:, 2:3],
                                                                      in1=gain[:], op0=ALU.mult, op1=ALU.mult),
                              reads=['ps0', 'ss%d' % i2, 'gain'], writes=['cb%d' % i2])
                    if not (KVAR & 128):
                        S_.op('act', lambda e: e.activation(out=vb[:, t, :, 0:64], in_=ps[0][:, 128:256].rearrange("p (g d) -> p g d", g=2),
                                                            func=AF.Copy), reads=['ps0'], writes=['vb'])
                    if not (KVAR & 2):
                        S_.op('act', lambda e: e.activation(out=absw[:, t, :], in_=ps[0][:, 256:264], func=AF.Abs),
                              reads=['ps0'], writes=['absw'])
                    if not (KVAR & 2):
                        S_.op('act', lambda e: e.activation(out=sgnw[:, t, :], in_=ps[0][:, 256:264], func=AF.Sign),
                              reads=['ps0'], writes=['sgnw'])
                    if not (KVAR & 8):
                        S_.op('pe', lambda e: e.transpose(self.pt[:, 0:128], cb[i2][:], self.ident[:]),
                              reads=['cb%d' % i2, 'ident'], writes=['pt'])
                    if not (KVAR & 8):
                        S_.op('act', lambda e: e.activation(out=cT[:, t * 128:(t + 1) * 128], in_=self.pt[:, 0:128], func=AF.Copy),
                              reads=['pt'], writes=['cT'])
                    if not (KVAR & 8):
                        S_.op('pe', lambda e: e.matmul(ps[1][:], cT[:, t * 128:(t + 1) * 128], wuv[:].rearrange("p h d -> p (h d)"),
                                                       start=True, stop=True), reads=['cT', 'wuv'], writes=['ps1'])
                    if not (KVAR & 8):
                        S_.op('dve', lambda e: e.tensor_copy(out=vA[:, t, :, 0:64], in_=ps[1][:].rearrange("p (h d) -> p h d", h=8)), reads=['ps1'], writes=['vA'])
                for cc in range(4 if not (KVAR & 16) else 0):
                    hk = 'hT%d' % cc
                    sl = slice(cc * 512, (cc + 1) * 512)
                    for kc in range(KC):
                        S_.op('pe', lambda e: e.matmul(ps[2][:], wki2[:, kc, :], self.hT[:, kc, sl],
                                                       start=(kc == 0), stop=(kc == KC - 1)), reads=[hk, 'wki2'], writes=['ps2'])
                    S_.op('act', lambda e: e.activation(out=kiT2[:, sl], in_=ps[2][:], func=AF.Copy), reads=['ps2'], writes=['kiT2'])
                    for kc in range(KC):
                        S_.op('pe', lambda e: e.matmul(ps[3][:], wkb[:, kc, :], self.hT[:, kc, sl],
                                                       start=(kc == 0), stop=(kc == KC - 1)), reads=[hk, 'wkb'], writes=['ps3'])
                    S_.op('dve', lambda e: e.tensor_copy(out=kbT[:, sl], in_=ps[3][:]), reads=['ps3'], writes=['kbT'])
                S_.barrier()
            if STOP == 2:
                return
            qlatT = self.sb(st, 'qlatT', [128, 8, 512], BF16)
            qa_t = [self.sb(st, 'qa_t0', [128, 512], BF16)] * 2
            qiT = self.sb(st, 'qiT', [128, 4, 512], BF16)
            qbT = self.sb(st, 'qbT', [128, 4, 512], BF16)
            score = self.sb(st, 'score', [128, S], F32)
            maskb = self.sb(st, 'maskb', [128, S], BF16)
            rtmp = [self.sb(st, 'rtmp%d' % i, [128, 512], F32) for i in range(2)]
            nm_off = self.sb(st, 'nm_off', [128, 12, 512], BF16)
            nm_dg = self.sb(st, 'nm_dg', [128, 4, 512], BF16)
            pT = [self.sb(st, 'pT%d' % i, [128, 512], BF16) for i in range(2)]
            rd = self.sb(st, 'rd', [128, 512], F32)
            aoT = self.sb(st, 'aoT', [128, KC, 512], BF16)
            bs = self.sb(st, 'bs', [128, 8], F32)
            Dk = self.sb(st, 'Dk', [128, NIT], F32)
            self.ln_setup(st, 0, 0, alias=(score, 'score'))
            S_.op('pool', lambda e: e.memset(nm_dg[:], -1024.0), writes=['nm_dg'])
            sc = 0.125
            pcnt = 0
            for c in range(4):
                hk = 'hT%d' % c
                qsl = slice(c * 512, (c + 1) * 512)
                for j in range(4):
                    qt = qa_t[j % 2]
                    qk = 'qa_t0'
                    for kc in range(KC):
                        S_.op('pe', lambda e: e.matmul(ps[0][:], wqa[:, kc, j * 128:(j + 1) * 128], self.hT[:, kc, qsl],
                                                       start=(kc == 0), stop=(kc == KC - 1)), reads=[hk, 'wqa'], writes=['ps0'])
                    S_.op('act', lambda e: e.activation(out=qt[:], in_=ps[0][:], func=AF.Copy), reads=['ps0'], writes=[qk])
                    for hh in range(2):
                        h = 2 * j + hh
                        rows = slice(hh * 64, (hh + 1) * 64)
                        S_.op('pe', lambda e: e.matmul(ps[1][:], wuk[rows, j, :], qt[rows, :], start=True, stop=True),
                              reads=[qk, 'wuk'], writes=['ps1'])
                        S_.op('dve', lambda e: e.tensor_copy(out=qlatT[:, h, :], in_=ps[1][:]), reads=['ps1'], writes=['qlatT'])
                    for kc in range(KC):
                        S_.op('pe', lambda e: e.matmul(ps[0][:], wqi[:, kc, j * 128:(j + 1) * 128], self.hT[:, kc, qsl],
                                                       start=(kc == 0), stop=(kc == KC - 1)), reads=[hk, 'wqi'], writes=['ps0'])
                    S_.op('act', lambda e: e.activation(out=qiT[:, j, :], in_=ps[0][:], func=AF.Copy), reads=['ps0'], writes=['qiT'])
                    for kc in range(KC):
                        S_.op('pe', lambda e: e.matmul(ps[1][:], wqb[:, kc, j * 128:(j + 1) * 128], self.hT[:, kc, qsl],
                                                       start=(kc == 0), stop=(kc == KC - 1)), reads=[hk, 'wqb'], writes=['ps1'])
                    S_.op('dve', lambda e: e.tensor_copy(out=qbT[:, j, :], in_=ps[1][:]), reads=['ps1'], writes=['qbT'])
                if STOP == 3:
                    S_.barrier()
                    return
                for tt in range(4):
                    tq = 4 * c + tt
                    nk = (tq + 1) * 128
                    for kch in range((nk + 511) // 512):
                        k0 = kch * 512
                        ncol = min(512, nk - k0)
                        for hi in range(8):
                            j, hh = hi // 2, hi % 2
                            rows = slice(hh * 64, (hh + 1) * 64)
                            pj = pcnt % 2
                            pcnt += 1
                            S_.op('pe', lambda e: e.matmul(ps[pj][:, 0:ncol], qiT[rows, j, tt * 128:(tt + 1) * 128],
                                                           kiT2[rows, k0:k0 + ncol], start=True, stop=True),
                                  reads=['qiT', 'kiT2'], writes=['ps%d' % pj])
                            S_.op('act', lambda e: e.activation(out=rtmp[pj][:, 0:ncol], in_=ps[pj][:, 0:ncol], func=AF.Relu,
                                                                scale=absw[:, tq, hi:hi + 1]),
                                  reads=['ps%d' % pj, 'absw'], writes=['rtmp%d' % pj])
                            if hi == 0:
                                S_.op('dve', lambda e: e.tensor_scalar(out=score[:, k0:k0 + ncol], in0=rtmp[pj][:, 0:ncol],
                                                                       scalar1=sgnw[:, tq, 0:1], scalar2=None, op0=ALU.mult),
                                      reads=['rtmp%d' % pj, 'sgnw'], writes=['score'])
                            else:
                                S_.op('dve', lambda e: e.scalar_tensor_tensor(out=score[:, k0:k0 + ncol], in0=rtmp[pj][:, 0:ncol],
                                                                              scalar=sgnw[:, tq, hi:hi + 1],
                                                                              in1=score[:, k0:k0 + ncol], op0=ALU.mult, op1=ALU.add),
                                      reads=['rtmp%d' % pj, 'sgnw', 'score'], writes=['score'])
                    if tq >= 2:
                        S_.op('dve', lambda e: e.tensor_reduce(out=bs[:, 0:1], in_=score[:, 0:nk], axis=AX.X, op=ALU.max,
                                                               apply_absolute_value=True), reads=['score'], writes=['bs'])
                    S_.op('dve', lambda e: e.tensor_tensor(out=score[:, tq * 128:nk], in0=score[:, tq * 128:nk], in1=tri[:],
                                                           op=ALU.add), reads=['score', 'tri'], writes=['score'])
                    if tq >= 2:
                        S_.op('dve', lambda e: e.tensor_scalar(out=Dk[:], in0=pw[:], scalar1=bs[:, 0:1], scalar2=None, op0=ALU.mult),
                              reads=['bs', 'pw'], writes=['Dk'])
                        S_.op('dve', lambda e: e.tensor_scalar(out=bs[:, 1:2], in0=bs[:, 0:1], scalar1=-1.0, scalar2=None,
                                                               op0=ALU.mult), reads=['bs'], writes=['bs'])
                        for it in range(NIT):
                            S_.op('dve', lambda e: e.tensor_tensor(out=bs[:, 2:3], in0=bs[:, 1:2], in1=Dk[:, it:it + 1], op=ALU.add),
                                  reads=['bs', 'Dk'], writes=['bs'])
                            S_.op('dve', lambda e: e.tensor_scalar(out=maskb[:, 0:nk], in0=score[:, 0:nk], scalar1=bs[:, 2:3],
                                                                   scalar2=0.0, op0=ALU.is_ge, op1=ALU.add, accum_out=bs[:, 3:4]),
                                  reads=['score', 'bs'], writes=['maskb', 'bs'])
                            S_.op('dve', lambda e: e.scalar_tensor_tensor(out=bs[:, 4:5], in0=bs[:, 3:4], scalar=255.5,
                                                                          in1=Dk[:, it:it + 1], op0=ALU.is_ge, op1=ALU.mult),
                                  reads=['bs', 'Dk'], writes=['bs'])
                            S_.op('dve', lambda e: e.tensor_tensor(out=bs[:, 1:2], in0=bs[:, 1:2], in1=bs[:, 4:5], op=ALU.add),
                                  reads=['bs'], writes=['bs'])
                    else:
                        S_.op('dve', lambda e: e.memset(bs[:, 1:2], -1e29), writes=['bs'])
                    S_.op('dve', lambda e: e.tensor_scalar(out=maskb[:, 0:nk], in0=score[:, 0:nk], scalar1=bs[:, 1:2],
                                                           scalar2=-1024.0, op0=ALU.is_lt, op1=ALU.mult),
                          reads=['score', 'bs'], writes=['maskb'])
                    kt = 0
                    while kt <= tq:
                        lim = 4 * c if kt < 4 * c else tq + 1
                        n = min(8, lim - kt)
                        for i in range(n):
                            S_.op('pe', lambda e: e.transpose(self.pt[:, i * 128:(i + 1) * 128],
                                                              maskb[:, (kt + i) * 128:(kt + i + 1) * 128], self.ident[:]),
                                  reads=['maskb', 'ident'], writes=['pt'])
                        src = self.pt[:, 0:n * 128].rearrange("p (k n) -> p k n", k=n)
                        if kt < 4 * c:
                            dst = nm_off[:, kt:kt + n, tt * 128:(tt + 1) * 128]
                            S_.op('act', lambda e: e.activation(out=dst, in_=src, func=AF.Copy), reads=['pt'], writes=['nm_off'])
                        else:
                            dst = nm_dg[:, kt - 4 * c:kt - 4 * c + n, tt * 128:(tt + 1) * 128]
                            S_.op('act', lambda e: e.activation(out=dst, in_=src, func=AF.Copy), reads=['pt'], writes=['nm_dg'])
                        kt += n
                if STOP == 4:
                    S_.barrier()
                    return
                nkt = 4 * c + 4
                blocks = []
                for h in range(8):
                    j, hh = h // 2, h % 2
                    rows = slice(hh * 64, (hh + 1) * 64)
                    for kt in range(nkt):
                        def qk(sT, sk, pj, h=h, kt=kt):
                            r = kt - 4 * c
                            near = r >= -1
                            nm = nm_off[:, kt, :] if kt < 4 * c else nm_dg[:, r, :]
                            nmk = 'nm_off' if kt < 4 * c else 'nm_dg'
                            S_.op('pe', lambda e: e.matmul(sT[:], cT[:, kt * 128:(kt + 1) * 128], qlatT[:, h, :], start=True, stop=False),
                                  reads=['cT', 'qlatT'], writes=[sk])
                            S_.op('pe', lambda e: e.matmul(sT[:], self.ident[:], nm, start=False, stop=(not near)),
                                  reads=['ident', nmk], writes=[sk])
                            if near:
                                c0, c1 = max(r, 0) * 128, min(r + 2, 4) * 128
                                tb0 = 0 if r >= 0 else 128
                                S_.op('pe', lambda e: e.matmul(sT[:, c0:c1], self.ident[:], TB[:, h, tb0:tb0 + (c1 - c0)],
                                                               start=False, stop=True), reads=['ident', 'TB'], writes=[sk])
                            S_.op('act', lambda e: e.activation(out=pT[pj][:], in_=sT[:], func=AF.Exp, bias=cfar[:, h:h + 1], scale=sc),
                                  reads=[sk, 'cfar'], writes=['pT%d' % pj])

                        def pv(pj, h=h, kt=kt):
                            ap_, akey = ps[4 + h % 2], 'ps%d' % (4 + h % 2)
                            S_.op('pe', lambda e: e.matmul(ap_[:], vA[:, kt, h, :], pT[pj][:], start=(kt == 0), stop=(kt == nkt - 1)),
                                  reads=['vA', 'pT%d' % pj], writes=[akey])

                        def post(h=h, j=j, rows=rows):
                            ap_, akey = ps[4 + h % 2], 'ps%d' % (4 + h % 2)
                            rk = 'rd%d' % (h % 2)
                            S_.op('dve', lambda e: e.reciprocal(out=rd[rows, :], in_=ap_[64:128, :]), reads=[akey], writes=[rk])
                            S_.op('dve', lambda e: e.tensor_tensor(out=aoT[rows, j, :], in0=rd[rows, :], in1=ap_[0:64, :], op=ALU.mult),
                                  reads=[rk, akey], writes=['aoT%d' % (h % 2)])
                        blocks.append({'qk': qk, 'pv': pv, 'post': post if kt == nkt - 1 else None})
                for hb in range(8):
                    g, j = hb // 4, hb % 4
                    krows = slice(g * 64, (g + 1) * 64)
                    orows = slice((hb % 2) * 64, (hb % 2 + 1) * 64)
                    kts = [kt for kt in range(4 * c - 1, 4 * c + 4) if kt >= 0]
                    for kt in kts:
                        r = kt - 4 * c
                        c0, c1 = max(r, 0) * 128, min(r + 2, 4) * 128
                        tb0 = 0 if r >= 0 else 128

                        def qk(sT, sk, pj, hb=hb, kt=kt, c0=c0, c1=c1, tb0=tb0, krows=krows, j=j):
                            S_.op('pe', lambda e: e.matmul(sT[:, c0:c1], kbT[krows, kt * 128:(kt + 1) * 128], qbT[krows, j, c0:c1],
                                                           start=True, stop=False), reads=['kbT', 'qbT'], writes=[sk])
                            S_.op('pe', lambda e: e.matmul(sT[:, c0:c1], self.ident[:], TB[:, 8 + hb, tb0:tb0 + (c1 - c0)],
                                                           start=False, stop=True), reads=['ident', 'TB'], writes=[sk])
                            S_.op('act', lambda e: e.activation(out=pT[pj][:, c0:c1], in_=sT[:, c0:c1], func=AF.Exp, scale=sc),
                                  reads=[sk], writes=['pT%d' % pj])

                        def pv(pj, hb=hb, kt=kt, c0=c0, c1=c1, g=g):
                            ap_, akey = ps[4 + hb % 2], 'ps%d' % (4 + hb % 2)
                            for qq in range(c0 // 128, c1 // 128):
                                tqq = 4 * c + qq
                                first = (kt == max(tqq - 1, 0))
                                last = (kt == tqq)
                                qs = slice(qq * 128, (qq + 1) * 128)
                                S_.op('pe', lambda e: e.matmul(ap_[:, qs], vb[:, kt, g, :], pT[pj][:, qs],
                                                               start=first, stop=last), reads=['vb', 'pT%d' % pj], writes=[akey])

                        def post(hb=hb, orows=orows):
                            m2 = hb // 2
                            ap_, akey = ps[4 + hb % 2], 'ps%d' % (4 + hb % 2)
                            rk = 'rd%d' % (hb % 2)
                            S_.op('dve', lambda e: e.tensor_scalar(out=rd[orows, :], in0=ap_[64:128, :], scalar1=esink2[orows, m2:m2 + 1], scalar2=None,
                                                                   op0=ALU.add), reads=[akey, 'esink2'], writes=[rk])
                            S_.op('dve', lambda e: e.reciprocal(out=rd[orows, :], in_=rd[orows, :]), reads=[rk], writes=[rk])
                            S_.op('dve', lambda e: e.tensor_tensor(out=aoT[orows, 4 + m2, :], in0=rd[orows, :], in1=ap_[0:64, :], op=ALU.mult),
                                  reads=[rk, akey], writes=['aoT%d' % (hb % 2)])
                        blocks.append({'qk': qk, 'pv': pv, 'post': post if kt == kts[-1] else None})
                self.run_blocks(blocks)
                if STOP == 6:
                    S_.barrier()
                    return
                for tt in range(4):
                    for hf in range(2):
                        for j in range(KC):
                            S_.op('pe', lambda e: e.matmul(ps[hf][:], aoT[:, j, tt * 128:(tt + 1) * 128],
                                                           wout[:, j, hf * 512:(hf + 1) * 512], start=(j == 0), stop=(j == KC - 1)),
                                  reads=['aoT0', 'aoT1', 'wout'], writes=['ps%d' % hf])
                    self.ln_tile([ps[0][:], ps[1][:]], ['ps0', 'ps1'], 4 * c + tt, res_loc, out_loc)
            S_.barrier()

    def phase_mix1(self, b, res_loc, out_loc):
        S_, ps, nc = self.S, self.ps, self.nc
        W = self.t['od_w_in']
        r3 = lambda a: a.rearrange("(k p) n -> p k n", p=128)
        sc = 0.125
        with ExitStack() as st:
            wq = self.sb(st, 'wq', [128, KC, D], BF16)
            wgl = self.sb(st, 'wgl', [128, KC, 48], BF16)
            wout = self.sb(st, 'wout', [128, KC, D], BF16)
            kcmpT = self.sb(st, 'kcmpT', [128, 2, 128], BF16)
            vcmp = self.sb(st, 'vcmp', [128, 2, 128], BF16)
            ksE = [self.sb(st, 'ksE%d' % g, [128, S], BF16) for g in range(2)]
            kwT = self.sb(st, 'kwT', [128, 2, S], BF16)
            vs = self.sb(st, 'vs', [128, NT, 2, 128], BF16)
            vw = self.sb(st, 'vw', [128, NT, 2, 128], BF16)
            TB = self.sb(st, 'TB', [128, 16, 256], BF16)
            M4 = self.sb(st, 'M4', [128, 128], BF16)
            cfar = self.sb(st, 'cfar', [128, 16], F32)
            OV = self.sb(st, 'OV', [128, 33], BF16)
            gsel = self.sb(st, 'gsel', [48, 24, 128], BF16)
            zeros = self.sb(st, 'zeros', [128, 128], BF16)
            for hf in range(2):
                for j in range(8):
                    c0 = (hf * 8 + j) * 64
                    self.load_w(wq[:, :, j * 128 + hf * 64:j * 128 + (hf + 1) * 64], r3(W[:, c0:c0 + 64]), 'wq')
            self.load_w(wgl[:], r3(W[:, 1792:1840]), 'wgl')
            self.load_w(wout[:], r3(self.t['od_w_out']), 'wout')
            self.load_w(OV[0:127, :], self.t['ovl'], 'OV')
            self.load_w(gsel[:], self.t['gsel'], 'gsel')
            S_.dma('sp', cfar[:], self.bcast_rows(self.t['cfar'][0:1, :], 16), writes=['cfar'])
            S_.op('pool', lambda e: e.memset(zeros[:], 0.0), writes=['zeros'])
            S_.op('pool', lambda e: e.memset(kwT[:], 0.0), writes=['kwT'])
            S_.op('pool', lambda e: e.memset(kcmpT[:], 0.0), writes=['kcmpT'])
            for g in range(2):
                S_.op('pool', lambda e: e.memset(ksE[g][96:128, :], 0.0), writes=['ksE%d' % g])
            S_.op('pool', lambda e: e.memset(vs[:], 1.0), writes=['vs'])
            S_.op('pool', lambda e: e.memset(vw[:], 1.0), writes=['vw'])
            S_.op('pool', lambda e: e.memset(vcmp[:], 1.0), writes=['vcmp'])
            for g in range(2):
                self.load_w(ksE[g][64:96, :], self.t['eblk'], 'ksE%d' % g)
            S_.op('pool', lambda e: e.memset(M4[:], 0.0), writes=['M4'])
            S_.op('pool', lambda e: e.affine_select(out=M4[:], in_=M4[:], pattern=[[-1, 128]], compare_op=ALU.is_ge,
                                                      fill=NEG, base=-1, channel_multiplier=1), reads=['M4'], writes=['M4'])
            with ExitStack() as st2:
                bt = [self.sb(st2, 'bt%d' % i, [128, 256], F32) for i in range(2)]
                cm = self.sb(st2, 'cm', [128, 256], F32)
                S_.op('pool', lambda e: e.memset(cm[:], 0.0), writes=['cm'])
                S_.op('pool', lambda e: e.affine_select(out=cm[:, 0:128], in_=cm[:, 0:128], pattern=[[1, 128]], compare_op=ALU.is_ge,
                                                          fill=NEG, base=0, channel_multiplier=-1), reads=['cm'], writes=['cm'])
                for h in range(16):
                    k = 'bt%d' % (h % 2)
                    S_.dma('sp', bt[h % 2][:], self.t['bt_tab'][h], writes=[k])
                    S_.op('dve', lambda e: e.tensor_scalar(out=bt[h % 2][:], in0=bt[h % 2][:], scalar1=cfar[:, h:h + 1],
                                                           scalar2=8.0, op0=ALU.subtract, op1=ALU.mult),
                          reads=[k, 'cfar'], writes=[k])
                    S_.op('dve', lambda e: e.tensor_tensor(out=TB[:, h, :], in0=bt[h % 2][:], in1=cm[:], op=ALU.add),
                          reads=[k, 'cm'], writes=['TB'])
                S_.barrier()
            with ExitStack() as st2:
                wkf = self.sb(st2, 'wkf', [128, KC, 512], BF16)
                wtm = self.sb(st2, 'wtm', [128, KC, 256], BF16)
                w1 = [self.sb(st2, 'w1_%d' % i, [128, 32, 128], BF16) for i in range(2)]
                w2 = [self.sb(st2, 'w2_%d' % i, [128, 64], BF16) for i in range(2)]
                pe_sb = [self.sb(st2, 'pe%d' % i, [32, 64], BF16) for i in range(2)]
                peT = [self.sb(st2, 'peT%d' % i, [64, 32], BF16) for i in range(2)]
                crT = [self.sb(st2, 'crT%d' % i, [128, S], BF16) for i in range(2)]
                cpe = self.sb(st2, 'cpe', [128, 2], F32)
                u = self.sb(st2, 'u', [128, 128], F32)
                u2 = self.sb(st2, 'u2', [128, 128], F32)
                th = self.sb(st2, 'th', [128, 128], F32)
                hb_ = self.sb(st2, 'hidb', [128, 128], BF16)
                self.load_w(wkf[:, :, 0:256], r3(W[:, 1024:1280]), 'wkf')
                self.load_w(wkf[:, :, 256:384], r3(W[:, 1280:1408]), 'wkf')
                self.load_w(wkf[:, :, 384:512], r3(W[:, 1536:1664]), 'wkf')
                self.load_w(wtm[:, :, 0:128], r3(W[:, 1408:1536]), 'wtm')
                self.load_w(wtm[:, :, 128:256], r3(W[:, 1664:1792]), 'wtm')
                for i, nm in enumerate(('k', 'v')):
                    for g in range(2):
                        self.load_w(w1[i][g * 64:(g + 1) * 64, :, :],
                                    self.t['od_w1_' + nm].rearrange("(l d) j -> d l j", d=64), 'w1_%d' % i)
                    self.load_w(w2[i][:], self.t['od_w2_' + nm], 'w2_%d' % i)
                    self.load_w(pe_sb[i][:], self.t['od_pe_' + nm], 'pe%d' % i)
                    S_.op('pe', lambda e: e.transpose(self.pt[0:64, 0:32], pe_sb[i][:], self.ident[0:32, 0:32]),
                          reads=['pe%d' % i, 'ident'], writes=['pt'])
                    S_.op('dve', lambda e: e.tensor_copy(out=peT[i][:], in_=self.pt[0:64, 0:32]), reads=['pt'], writes=['peT%d' % i])
                    for l in range(32):
                        S_.op('pe', lambda e: e.matmul(ps[6][:, i:i + 1], w1[i][0:64, l, :], peT[i][:, l:l + 1],
                                                       start=(l == 0), stop=(l == 31)), reads=['w1_%d' % i, 'peT%d' % i], writes=['ps6'])
                    S_.op('dve', lambda e: e.tensor_copy(out=cpe[:, i:i + 1], in_=ps[6][:, i:i + 1]), reads=['ps6'], writes=['cpe'])
                for t in range(NT):
                    hk = 'hT%d' % (t // 4)
                    for kc in range(KC):
                        S_.op('pe', lambda e: e.matmul(ps[0][:, 0:256], self.hT[:, kc, t * 128:(t + 1) * 128], wtm[:, kc, :],
                                                       start=(kc == 0), stop=(kc == KC - 1)), reads=[hk, 'wtm'], writes=['ps0'])
                    S_.op('act', lambda e: e.activation(out=vs[:, t, :, 0:64], in_=ps[0][:, 0:128].rearrange("p (g d) -> p g d", g=2),
                                                        func=AF.Copy), reads=['ps0'], writes=['vs'])
                    S_.op('dve', lambda e: e.tensor_copy(out=vw[:, t, :, 0:64], in_=ps[0][:, 128:256].rearrange("p (g d) -> p g d", g=2)),
                          reads=['ps0'], writes=['vw'])
                dsts = [(crT[0], 'crT0'), (crT[1], 'crT1'), (None, 'ks'), (None, 'kw')]
                n_ = 0
                for cc in range(4):
                    hk = 'hT%d' % cc
                    sl = slice(cc * 512, (cc + 1) * 512)
                    for gi, (dst, dk) in enumerate(dsts):
                        pj = 1 + (n_ % 2)
                        n_ += 1
                        for kc in range(KC):
                            S_.op('pe', lambda e: e.matmul(ps[pj][:], wkf[:, kc, gi * 128:(gi + 1) * 128], self.hT[:, kc, sl],
                                                           start=(kc == 0), stop=(kc == KC - 1)), reads=[hk, 'wkf'], writes=['ps%d' % pj])
                        if dk == 'ks':
                            S_.op('act', lambda e: e.activation(out=ksE[0][0:64, sl], in_=ps[pj][0:64, :], func=AF.Copy), reads=['ps%d' % pj], writes=['ksE0'])
                            S_.op('act', lambda e: e.activation(out=ksE[1][0:64, sl], in_=ps[pj][64:128, :], func=AF.Copy), reads=['ps%d' % pj], writes=['ksE1'])
                        elif dk == 'kw':
                            S_.op('dve', lambda e: e.tensor_copy(out=kwT[0:64, 0, sl], in_=ps[pj][0:64, :]), reads=['ps%d' % pj], writes=['kwT'])
                            S_.op('dve', lambda e: e.tensor_copy(out=kwT[64:128, 1, sl], in_=ps[pj][64:128, :]), reads=['ps%d' % pj], writes=['kwT'])
                        elif gi % 2 == 0:
                            S_.op('act', lambda e: e.activation(out=dst[:, sl], in_=ps[pj][:], func=AF.Copy), reads=['ps%d' % pj], writes=[dk])
                        else:
                            S_.op('dve', lambda e: e.tensor_copy(out=dst[:, sl], in_=ps[pj][:]), reads=['ps%d' % pj], writes=[dk])
                for i in range(2):
                    for g in range(2):
                        rows = slice(g * 64, (g + 1) * 64)
                        for l in range(32):
                            S_.op('pe', lambda e: e.matmul(ps[3][:, 0:127], w1[i][rows, l, :], crT[i][rows, l:l + 16 * 126 + 1:16],
                                                           start=(l == 0), stop=(l == 31)), reads=['w1_%d' % i, 'crT%d' % i], writes=['ps3'])
                        S_.op('act', lambda e: e.activation(out=u[:, 0:127], in_=ps[3][:, 0:127], func=AF.Identity, bias=cpe[:, i:i + 1]),
                              reads=['ps3', 'cpe'], writes=['u'])
                        S_.op('dve', lambda e: e.tensor_tensor(out=u2[:, 0:127], in0=u[:, 0:127], in1=u[:, 0:127], op=ALU.mult),
                              reads=['u'], writes=['u2'])
                        S_.op('dve', lambda e: e.tensor_scalar(out=u2[:, 0:127], in0=u2[:, 0:127], scalar1=0.044715, scalar2=1.0,
                                                               op0=ALU.mult, op1=ALU.add), reads=['u2'], writes=['u2'])
                        S_.op('dve', lambda e: e.tensor_tensor(out=u2[:, 0:127], in0=u2[:, 0:127], in1=u[:, 0:127], op=ALU.mult),
                              reads=['u', 'u2'], writes=['u2'])
                        S_.op('act', lambda e: e.activation(out=th[:, 0:127], in_=u2[:, 0:127], func=AF.Tanh, scale=0.7978845608028654),
                              reads=['u2'], writes=['th'])
                        S_.op('dve', lambda e: e.scalar_tensor_tensor(out=th[:, 0:127], in0=th[:, 0:127], scalar=1.0, in1=u[:, 0:127],
                                                                      op0=ALU.add, op1=ALU.mult), reads=['th', 'u'], writes=['th'])
                        S_.op('dve', lambda e: e.tensor_scalar(out=hb_[:, 0:127], in0=th[:, 0:127], scalar1=0.5, scalar2=None, op0=ALU.mult),
                              reads=['th'], writes=['hidb'])
                        if i == 0:
                            S_.op('pe', lambda e: e.matmul(ps[4][rows, 0:127], w2[0][:], hb_[:, 0:127], start=True, stop=True),
                                  reads=['w2_0', 'hidb'], writes=['ps4'])
                            S_.op('act', lambda e: e.activation(out=kcmpT[rows, g, 0:127], in_=ps[4][rows, 0:127], func=AF.Copy),
                                  reads=['ps4'], writes=['kcmpT'])
                        else:
                            S_.op('pe', lambda e: e.matmul(ps[5][0:127, g * 64:(g + 1) * 64], hb_[:, 0:127], w2[1][:], start=True, stop=True),
                                  reads=['w2_1', 'hidb'], writes=['ps5'])
                            S_.op('act', lambda e: e.activation(out=vcmp[0:127, g, 0:64], in_=ps[5][0:127, g * 64:(g + 1) * 64], func=AF.Copy),
                                  reads=['ps5'], writes=['vcmp'])
                S_.barrier()
            qT = self.sb(st, 'qT', [128, 8, 512], BF16)
            qS = self.sb(st, 'qS', [128, 16, 512], BF16)
            S_.op('pool', lambda e: e.memset(qS[96:128, :, :], 0.0), writes=['qS'])
            gT = self.sb(st, 'gT', [48, 512], BF16)
            gsb = [self.sb(st, 'gsb%d' % i, [128, 512], F32) for i in range(2)]
            cbias = [self.sb(st, 'cbias%d' % i, [128, 512], F32) for i in range(2)]
            s_sb = [self.sb(st, 's_sb%d' % i, [128, 512], F32) for i in range(2)]
            pT = [self.sb(st, 'pT%d' % i, [128, 512], BF16) for i in range(2)]
            rd = self.sb(st, 'rd', [128, 512], F32)
            to = self.sb(st, 'to', [128, 512], F32)
            acc = self.sb(st, 'acc', [128, 8, 512], F32)
            aoT = self.sb(st, 'aoT', [128, 8, 512], BF16)
            pslc = self.sb(st, 'pslc', [128, 4, 64], F32)
            stab = self.sb(st, 'stab', [128, 3, 4, 64], F32)
            sscore = self.sb(st, 'sscore', [128, 64], F32)
            top = self.sb(st, 'top', [128, 16], F32)
            rq = self.sb(st, 'rq', [128, 8], F32)
            nsel = self.sb(st, 'nsel', [128, 64], BF16)
            self.ln_setup(st, 1, 0)
            pcnt = 0
            for c in range(4):
                hk = 'hT%d' % c
                qsl = slice(c * 512, (c + 1) * 512)
                for k3 in range(3):
                    S_.dma('sp', stab[:, k3, :, :], self.t['seltab'][k3, 4 * c:4 * c + 4].rearrange("t p n -> p t n"), writes=['stab'])
                for j in range(8):
                    pj = j % 2
                    for kc in range(KC):
                        S_.op('pe', lambda e: e.matmul(ps[pj][:], wq[:, kc, j * 128:(j + 1) * 128], self.hT[:, kc, qsl],
                                                       start=(kc == 0), stop=(kc == KC - 1)), reads=[hk, 'wq'], writes=['ps%d' % pj])
                    if j % 2 == 0:
                        S_.op('act', lambda e: e.activation(out=qT[:, j, :], in_=ps[pj][:], func=AF.Copy), reads=['ps%d' % pj], writes=['qT'])
                    else:
                        S_.op('dve', lambda e: e.tensor_copy(out=qT[:, j, :], in_=ps[pj][:]), reads=['ps%d' % pj], writes=['qT'])
                    S_.op('dve', lambda e: e.tensor_copy(out=qS[0:64, j, :], in_=ps[pj][0:64, :]), reads=['ps%d' % pj], writes=['qS'])
                    S_.op('act', lambda e: e.activation(out=qS[0:64, 8 + j, :], in_=ps[pj][64:128, :], func=AF.Copy), reads=['ps%d' % pj], writes=['qS'])
                for kc in range(KC):
                    S_.op('pe', lambda e: e.matmul(ps[0][0:48, :], wgl[:, kc, :], self.hT[:, kc, qsl],
                                                   start=(kc == 0), stop=(kc == KC - 1)), reads=[hk, 'wgl'], writes=['ps0'])
                S_.op('act', lambda e: e.activation(out=gT[:], in_=ps[0][0:48, :], func=AF.Copy), reads=['ps0'], writes=['gT'])

                def gate_pair(m, br):
                    gi = (m * 3 + br) % 2
                    S_.op('pe', lambda e: e.matmul(ps[0][:], gsel[:, m * 3 + br, :], gT[:], start=True, stop=True),
                          reads=['gsel', 'gT'], writes=['ps0'])
                    S_.op('act', lambda e: e.activation(out=gsb[gi][:], in_=ps[0][:], func=AF.Exp, scale=-1.0), reads=['ps0'], writes=['gsb%d' % gi])
                    return gi

                def finish_head(h, br, gi, acc_ps, acc_key, tiny):
                    m = h // 2
                    R = slice((h % 2) * 64, (h % 2 + 1) * 64)
                    ak = 'acc%d_%d' % (m, h % 2)
                    if tiny:
                        S_.op('dve', lambda e: e.tensor_scalar(out=rd[R, :], in0=acc_ps[64:128, :], scalar1=1e-30, scalar2=None, op0=ALU.add),
                              reads=[acc_key], writes=['rd%d' % (h % 2)])
                        S_.op('dve', lambda e: e.scalar_tensor_tensor(out=rd[R, :], in0=gsb[gi][R, :], scalar=1.0, in1=rd[R, :],
                                                                      op0=ALU.add, op1=ALU.mult),
                              reads=['gsb%d' % gi, 'rd%d' % (h % 2)], writes=['rd%d' % (h % 2)])
                    else:
                        S_.op('dve', lambda e: e.scalar_tensor_tensor(out=rd[R, :], in0=gsb[gi][R, :], scalar=1.0, in1=acc_ps[64:128, :],
                                                                      op0=ALU.add, op1=ALU.mult),
                              reads=['gsb%d' % gi, acc_key], writes=['rd%d' % (h % 2)])
                    S_.op('dve', lambda e: e.reciprocal(out=rd[R, :], in_=rd[R, :]), reads=['rd%d' % (h % 2)], writes=['rd%d' % (h % 2)])
                    if br == 0:
                        S_.op('dve', lambda e: e.tensor_tensor(out=acc[R, m, :], in0=rd[R, :], in1=acc_ps[0:64, :], op=ALU.mult),
                              reads=['rd%d' % (h % 2), acc_key], writes=[ak])
                    else:
                        S_.op('dve', lambda e: e.tensor_tensor(out=to[R, :], in0=rd[R, :], in1=acc_ps[0:64, :], op=ALU.mult),
                              reads=['rd%d' % (h % 2), acc_key], writes=['to%d' % (h % 2)])
                        S_.op('pool', lambda e: e.tensor_tensor(out=acc[R, m, :], in0=acc[R, m, :], in1=to[R, :], op=ALU.add),
                              reads=['to%d' % (h % 2), ak], writes=[ak])

                def head_info(h):
                    g = h // 8
                    return g, h % 8, slice(g * 64, (g + 1) * 64), slice((h % 2) * 64, (h % 2 + 1) * 64)

                blocks = []
                gstate = {}
                for m in range(8):
                    for hh in range(2):
                        h = 2 * m + hh
                        g, j, qrows, orows = head_info(h)

                        def qk(sT, sk, pj, h=h, g=g, j=j, qrows=qrows):
                            S_.dma('sp', cbias[pj][0:127, :], self.t['cb_tab'][h, c], writes=['cbias%d' % pj])
                            S_.op('pe', lambda e: e.matmul(sT[0:127, :], kcmpT[:, g, 0:127], qT[:, j, :], start=True, stop=True),
                                  reads=['kcmpT', 'qT'], writes=[sk])
                            S_.op('dve', lambda e: e.scalar_tensor_tensor(out=s_sb[pj][0:127, :], in0=sT[0:127, :], scalar=sc,
                                                                          in1=cbias[pj][0:127, :], op0=ALU.mult, op1=ALU.add),
                                  reads=[sk, 'cbias%d' % pj], writes=['s_sb%d' % pj])
                            S_.op('act', lambda e: e.activation(out=pT[pj][0:127, :], in_=s_sb[pj][0:127, :], func=AF.Exp),
                                  reads=['s_sb%d' % pj], writes=['pT%d' % pj])

                        def pv(pj, h=h, g=g, orows=orows):
                            ap_, akey = ps[4 + h % 2], 'ps%d' % (4 + h % 2)
                            S_.op('pe', lambda e: e.matmul(ap_[:], vcmp[0:127, g, :], pT[pj][0:127, :], start=True, stop=True),
                                  reads=['vcmp', 'pT%d' % pj], writes=[akey])
                            for tt in range(4):
                                S_.op('pe', lambda e: e.matmul(ps[1][:, tt * 64:tt * 64 + 33], pT[pj][0:127, tt * 128:(tt + 1) * 128], OV[0:127, :],
                                                               start=True, stop=True), reads=['OV', 'pT%d' % pj], writes=['ps1'])
                            S_.op('dve', lambda e: e.tensor_scalar(out=rq[:, 0:4], in0=ps[1][:].rearrange("p (t n) -> p t n", n=64)[:, 0:4, 32],
                                                                   scalar1=1e-30, scalar2=None, op0=ALU.add), reads=['ps1'], writes=['rq'])
                            S_.op('dve', lambda e: e.reciprocal(out=rq[:, 0:4], in_=rq[:, 0:4]), reads=['rq'], writes=['rq'])
                            for tt in range(4):
                                dst = pslc[:, tt, g * 32:(g + 1) * 32]
                                if h % 8 == 0:
                                    S_.op('dve', lambda e: e.tensor_scalar(out=dst, in0=ps[1][:, tt * 64:tt * 64 + 32], scalar1=rq[:, tt:tt + 1],
                                                                           scalar2=None, op0=ALU.mult), reads=['ps1', 'rq'], writes=['pslc'])
                                else:
                                    S_.op('dve', lambda e: e.scalar_tensor_tensor(out=dst, in0=ps[1][:, tt * 64:tt * 64 + 32], scalar=rq[:, tt:tt + 1],
                                                                                  in1=dst, op0=ALU.mult, op1=ALU.add),
                                          reads=['ps1', 'rq', 'pslc'], writes=['pslc'])

                        def post(h=h, m=m, hh=hh):
                            if hh == 0:
                                gstate['gi'] = gate_pair(m, 0)
                            finish_head(h, 0, gstate['gi'], ps[4 + h % 2], 'ps%d' % (4 + h % 2), True)
                        blocks.append({'qk': qk, 'pv': pv, 'post': post})
                self.run_blocks(blocks)
                for tt in range(4):
                    S_.op('dve', lambda e: e.tensor_tensor(out=sscore[:], in0=pslc[:, tt, :], in1=stab[:, 0, tt, :], op=ALU.add),
                          reads=['pslc', 'stab'], writes=['sscore'])
                    S_.op('dve', lambda e: e.tensor_tensor(out=sscore[:], in0=sscore[:], in1=stab[:, 1, tt, :], op=ALU.mult),
                          reads=['sscore', 'stab'], writes=['sscore'])
                    S_.op('dve', lambda e: e.tensor_tensor(out=sscore[:], in0=sscore[:], in1=stab[:, 2, tt, :], op=ALU.add),
                          reads=['sscore', 'stab'], writes=['sscore'])
                    for g in range(2):
                        S_.op('dve', lambda e: e.max(out=top[:, g * 8:(g + 1) * 8], in_=sscore[:, g * 32:(g + 1) * 32]),
                              reads=['sscore'], writes=['top'])
                    for g in range(2):
                        S_.op('dve', lambda e: e.tensor_scalar(out=nsel[:, g * 32:(g + 1) * 32], in0=sscore[:, g * 32:(g + 1) * 32],
                                                               scalar1=top[:, g * 8 + 7:g * 8 + 8], scalar2=-1024.0, op0=ALU.is_lt, op1=ALU.mult),
                              reads=['sscore', 'top'], writes=['nsel'])
                    for g in range(2):
                        S_.op('pe', lambda e: e.transpose(self.pt[64:96, g * 128:(g + 1) * 128], nsel[:, g * 32:(g + 1) * 32], self.ident[:]),
                              reads=['nsel', 'ident'], writes=['pt'])
                    for g in range(2):
                        src0 = self.pt[64:96, g * 128:(g + 1) * 128]
                        src = bass.AP(src0.tensor, src0.offset, [list(src0.ap[0]), [0, 8], [1, 128]])
                        if g == 0:
                            S_.op('act', lambda e: e.activation(out=qS[64:96, 0:8, tt * 128:(tt + 1) * 128], in_=src, func=AF.Copy),
                                  reads=['pt'], writes=['qS'])
                        else:
                            S_.op('dve', lambda e: e.tensor_copy(out=qS[64:96, 8:16, tt * 128:(tt + 1) * 128], in_=src),
                                  reads=['pt'], writes=['qS'])
                blocks = []
                for br in (1, 2):
                    for m in range(8):
                        for hh in range(2):
                            h = 2 * m + hh
                            g, j, qrows, orows = head_info(h)
                            kts = list(range(0, 4 * c + 4)) if br == 1 else list(range(max(4 * c - 4, 0), 4 * c + 4))
                            for ki, kt in enumerate(kts):
                                r = kt - 4 * c
                                lo = max(r, 0) * 128
                                hi = 512 if br == 1 else (min(r + 4, 3) + 1) * 128

                                def qk(sT, sk, pj, br=br, h=h, g=g, j=j, qrows=qrows, kt=kt, r=r, lo=lo, hi=hi):
                                    near = r >= -1
                                    m4 = (br == 2 and r <= -1)
                                    if br == 1:
                                        S_.op('pe', lambda e: e.matmul(sT[:, lo:hi], ksE[g][:, kt * 128:(kt + 1) * 128], qS[:, h, lo:hi],
                                                                       start=True, stop=(not near)), reads=['ksE%d' % g, 'qS'], writes=[sk])
                                    else:
                                        S_.op('pe', lambda e: e.matmul(sT[:, lo:hi], kwT[:, g, kt * 128:(kt + 1) * 128], qT[:, j, lo:hi],
                                                                       start=True, stop=(not near and not m4)), reads=['kwT', 'qT'], writes=[sk])
                                    if near:
                                        c0, c1 = max(r, 0) * 128, min(r + 2, 4) * 128
                                        tb0 = 0 if r >= 0 else 128
                                        S_.op('pe', lambda e: e.matmul(sT[:, c0:c1], self.ident[:], TB[:, h, tb0:tb0 + (c1 - c0)],
                                                                       start=False, stop=(not m4)), reads=['ident', 'TB'], writes=[sk])
                                    if m4:
                                        q4 = (r + 4) * 128
                                        S_.op('pe', lambda e: e.matmul(sT[:, q4:q4 + 128], self.ident[:], M4[:], start=False, stop=True),
                                              reads=['ident', 'M4'], writes=[sk])
                                    S_.op('act', lambda e: e.activation(out=pT[pj][:, lo:hi], in_=sT[:, lo:hi], func=AF.Exp, bias=cfar[:, h:h + 1], scale=sc),
                                          reads=[sk, 'cfar'], writes=['pT%d' % pj])

                                def pv(pj, br=br, h=h, g=g, kt=kt, lo=lo, hi=hi, ki=ki, nk_=len(kts)):
                                    vv = vs if br == 1 else vw
                                    vvn = 'vs' if br == 1 else 'vw'
                                    ap_, akey = ps[4 + h % 2], 'ps%d' % (4 + h % 2)
                                    full0 = (ki == 0 and lo == 0 and hi == 512)
                                    if ki == 0 and not full0:
                                        S_.op('pe', lambda e: e.matmul(ap_[:], zeros[:], qT[:, 0, :], start=True, stop=False),
                                              reads=['zeros', 'qT'], writes=[akey])
                                    lastk = (ki == nk_ - 1)
                                    S_.op('pe', lambda e: e.matmul(ap_[:, lo:hi], vv[:, kt, g, :], pT[pj][:, lo:hi],
                                                                   start=full0, stop=lastk), reads=[vvn, 'pT%d' % pj], writes=[akey])

                                def post(h=h, m=m, hh=hh, br=br):
                                    if hh == 0:
                                        gstate['gi'] = gate_pair(m, br)
                                    finish_head(h, br, gstate['gi'], ps[4 + h % 2], 'ps%d' % (4 + h % 2), False)
                                blocks.append({'qk': qk, 'pv': pv, 'post': post if ki == len(kts) - 1 else None})
                self.run_blocks(blocks)
                for m in range(8):
                    if m % 2 == 0:
                        S_.op('act', lambda e: e.activation(out=aoT[:, m, :], in_=acc[:, m, :], func=AF.Copy), reads=['acc%d_0' % m, 'acc%d_1' % m], writes=['aoT'])
                    else:
                        S_.op('dve', lambda e: e.tensor_copy(out=aoT[:, m, :], in_=acc[:, m, :]), reads=['acc%d_0' % m, 'acc%d_1' % m], writes=['aoT'])
                for tt in range(4):
                    for hf in range(2):
                        for j in range(KC):
                            S_.op('pe', lambda e: e.matmul(ps[hf][:], aoT[:, j, tt * 128:(tt + 1) * 128],
                                                           wout[:, j, hf * 512:(hf + 1) * 512], start=(j == 0), stop=(j == KC - 1)),
                                  reads=['aoT', 'wout'], writes=['ps%d' % hf])
                    self.ln_tile([ps[0][:], ps[1][:]], ['ps0', 'ps1'], 4 * c + tt, res_loc, out_loc)
            S_.barrier()

    def phase_ffn(self, b, res_loc, out_loc, moe):
        S_, ps = self.S, self.ps
        G = 4 if moe else 2
        nexp = N_EXP if moe else 1
        ngrp = (D_FFE if moe else D_FF) // (128 * G)
        with ExitStack() as st:
            yacc = self.sb(st, 'yacc', [128, NT, D], F32)
            wg = [self.sb(st, 'wg%d' % j, [128, KC, G * 128], BF16) for j in range(2)]
            wu = [self.sb(st, 'wu%d' % j, [128, KC, G * 128], BF16) for j in range(2)]
            wd = [self.sb(st, 'wd%d' % j, [128, G, D], BF16) for j in range(2)]
            if moe:
                wr = self.sb(st, 'wr', [128, KC, N_EXP], BF16)
                gate = self.sb(st, 'gate', [128, NT, N_EXP], F32)
                lg = self.sb(st, 'rlg', [128, 8], F32)
                top = self.sb(st, 'rtop', [128, 8], F32)
                wk_ = self.sb(st, 'rwk', [128, 8], F32)
                m1 = self.sb(st, 'rm1', [128, 8], F32)
                m2 = self.sb(st, 'rm2', [128, 8], F32)
                self.load_w(wr[:], self.t['moe_w_router'].rearrange("(k p) n -> p k n", p=128), 'wr')
                for t in range(NT):
                    for kc in range(KC):
                        S_.op('pe', lambda e: e.matmul(ps[6][:, 0:8], self.hT[:, kc, t * 128:(t + 1) * 128], wr[:, kc, :],
                                                       start=(kc == 0), stop=(kc == KC - 1)),
                              reads=['wr', 'hT%d' % (t // 4)], writes=['ps6'])
                    S_.op('dve', lambda e: e.tensor_copy(out=lg[:], in_=ps[6][:, 0:8]), reads=['ps6'], writes=['rlg'])
                    S_.op('dve', lambda e: e.max(out=top[:], in_=lg[:]), reads=['rlg'], writes=['rtop'])
                    S_.op('dve', lambda e: e.tensor_tensor(out=wk_[:, 0:1], in0=top[:, 0:1], in1=top[:, 1:2], op=ALU.subtract),
                          reads=['rtop'], writes=['rwk'])
                    S_.op('act', lambda e: e.activation(out=wk_[:, 1:2], in_=wk_[:, 0:1], func=AF.Sigmoid),
                          reads=['rwk'], writes=['rwk'])
                    S_.op('dve', lambda e: e.tensor_scalar(out=wk_[:, 2:3], in0=wk_[:, 1:2], scalar1=-1.0, scalar2=1.0,
                                                           op0=ALU.mult, op1=ALU.add), reads=['rwk'], writes=['rwk'])
                    S_.op('dve', lambda e: e.tensor_scalar(out=m1[:], in0=lg[:], scalar1=top[:, 0:1], scalar2=wk_[:, 1:2],
                                                           op0=ALU.is_equal, op1=ALU.mult), reads=['rlg', 'rtop', 'rwk'],
                          writes=['rm1'])
                    S_.op('dve', lambda e: e.tensor_scalar(out=m2[:], in0=lg[:], scalar1=top[:, 1:2], scalar2=wk_[:, 2:3],
                                                           op0=ALU.is_equal, op1=ALU.mult), reads=['rlg', 'rtop', 'rwk'],
                          writes=['rm2'])
                    S_.op('dve', lambda e: e.tensor_tensor(out=gate[:, t, :], in0=m1[:], in1=m2[:], op=ALU.add),
                          reads=['rm1', 'rm2'], writes=['gate'])

            def wsrc(ex, g):
                c0 = g * G * 128
                if moe:
                    return (self.t['moe_w_gate'][ex, :, c0:c0 + G * 128], self.t['moe_w_up'][ex, :, c0:c0 + G * 128],
                            self.t['moe_w_down'][ex, c0:c0 + G * 128, :])
                return (self.t['ff_w_gate'][:, c0:c0 + G * 128], self.t['ff_w_up'][:, c0:c0 + G * 128],
                        self.t['ff_w_down'][c0:c0 + G * 128, :])

            def load_group(idx, ex, g):
                j = idx % 2
                a, u_, d_ = wsrc(ex, g)
                self.load_w(wg[j][:], a.rearrange("(k p) n -> p k n", p=128), 'wg%d' % j)
                self.load_w(wu[j][:], u_.rearrange("(k p) n -> p k n", p=128), 'wu%d' % j)
                self.load_w(wd[j][:], d_.rearrange("(g p) n -> p g n", p=128), 'wd%d' % j)

            groups = [(ex, g) for ex in range(nexp) for g in range(ngrp)]
            sg5 = [self.sb(st, 'sg5_%d' % j_, [128, 512], F32) for j_ in range(2)]
            abuf = [self.sb(st, 'abuf%d' % j_, [128, G, 512], BF16) for j_ in range(2)]
            ptf = self.pt[:].bitcast(F32)
            ybanks = [ps[4][:], ps[5][:], ps[6][:], ptf]
            ykeys = ['ps4', 'ps5', 'ps6', 'pt']
            load_group(0, *groups[0])
            cnt = [0]
            for idx, (ex, g) in enumerate(groups):
                if idx + 1 < len(groups):
                    load_group(idx + 1, *groups[idx + 1])
                j = idx % 2

                def emit_gu(tb, ci):
                    pj = cnt[0] % 2
                    cnt[0] += 1
                    pg, pu = ps[2 * pj], ps[2 * pj + 1]
                    kg, ku = 'ps%d' % (2 * pj), 'ps%d' % (2 * pj + 1)
                    hk = 'hT%d' % tb
                    tok = slice(tb * 512, (tb + 1) * 512)
                    for kc in range(KC):
                        S_.op('pe', lambda e: e.matmul(pg[:], wg[j][:, kc, ci * 128:(ci + 1) * 128], self.hT[:, kc, tok],
                                                       start=(kc == 0), stop=(kc == KC - 1)), reads=['wg%d' % j, hk], writes=[kg])
                    for kc in range(KC):
                        S_.op('pe', lambda e: e.matmul(pu[:], wu[j][:, kc, ci * 128:(ci + 1) * 128], self.hT[:, kc, tok],
                                                       start=(kc == 0), stop=(kc == KC - 1)), reads=['wu%d' % j, hk], writes=[ku])
                    S_.op('act', lambda e: e.activation(out=sg5[pj][:], in_=pg[:], func=AF.Silu), reads=[kg], writes=['sg5_%d' % pj])
                    S_.op('dve', lambda e: e.tensor_tensor(out=abuf[tb % 2][:, ci, :], in0=sg5[pj][:], in1=pu[:], op=ALU.mult),
                          reads=['sg5_%d' % pj, ku], writes=['abuf%d' % (tb % 2)])

                def emit_down(tb, hp):
                    ab, abk = abuf[tb % 2], 'abuf%d' % (tb % 2)
                    for ci in range(G):
                        for tt in range(2):
                            col = (hp * 2 + tt) * 128
                            for hf in range(2):
                                bi = tt * 2 + hf
                                S_.op('pe', lambda e: e.matmul(ybanks[bi], ab[:, ci, col:col + 128], wd[j][:, ci, hf * 512:(hf + 1) * 512],
                                                               start=(ci == 0), stop=(ci == G - 1)), reads=[abk, 'wd%d' % j], writes=[ykeys[bi]])
                    for tt in range(2):
                        t = tb * 4 + hp * 2 + tt
                        for hf in range(2):
                            bi = tt * 2 + hf
                            src, sk = ybanks[bi], ykeys[bi]
                            dst = yacc[:, t, hf * 512:(hf + 1) * 512]
                            yk = 'yacc%d' % t
                            if moe:
                                if idx == 0:
                                    S_.op('dve', lambda e: e.tensor_scalar(out=dst, in0=src, scalar1=gate[:, t, ex:ex + 1],
                                                                           scalar2=None, op0=ALU.mult), reads=[sk, 'gate'], writes=[yk])
                                else:
                                    S_.op('dve', lambda e: e.scalar_tensor_tensor(out=dst, in0=src, scalar=gate[:, t, ex:ex + 1], in1=dst,
                                                                                  op0=ALU.mult, op1=ALU.add), reads=[sk, 'gate', yk], writes=[yk])
                            else:
                                if idx == 0:
                                    S_.op('dve', lambda e: e.tensor_copy(out=dst, in_=src), reads=[sk], writes=[yk])
                                else:
                                    S_.op('dve', lambda e: e.tensor_tensor(out=dst, in0=dst, in1=src, op=ALU.add), reads=[sk, yk], writes=[yk])

                pending = None
                for tb in range(4):
                    for ci in range(G):
                        emit_gu(tb, ci)
                        if pending is not None and ci == 0:
                            emit_down(pending, 0)
                        if pending is not None and ci == G // 2:
                            emit_down(pending, 1)
                    pending = tb
                emit_down(3, 0)
                emit_down(3, 1)
            self.ln_setup(st, 1 if moe else 0, 2)
            for t in range(NT):
                self.ln_tile([yacc[:, t, 0:512], yacc[:, t, 512:1024]], ['yacc%d' % t] * 2, t, res_loc, out_loc,
                             want_hT=(out_loc[0] != 'out'))
            S_.barrier()


_PROG_CACHE = {}

FULL_PHASES = ['x0', 'mix0', 'xattn0', 'ffn', 'mix1', 'xattn1', 'moe']


def make_inputs(inputs, phases=None):
    f = lambda a: np.ascontiguousarray(np.asarray(a, dtype=np.float32))
    rel = f(inputs['rel_table'])
    kk = np.arange(128)[:, None]
    jj_ = np.arange(256)[None, :]
    bt_tab = np.ascontiguousarray(rel[_rel_bucket_np(jj_ - kk), :].transpose(2, 0, 1))
    cfar = np.ascontiguousarray(rel[31:32, :])
    u = (np.arange(S)[None, :] - 16 * np.arange(127)[:, None] - 31)
    cb_tab = np.where(u[None] < 0, np.float32(NEG), rel[_rel_bucket_np(u), :].transpose(2, 0, 1)).astype(np.float32)
    cb_tab = np.ascontiguousarray(cb_tab.reshape(16, 127, 4, 512).transpose(0, 2, 1, 3))
    n = np.arange(127)[:, None] * 16
    jb = np.arange(32)[None, :] * 64
    ovl = np.concatenate([((n < jb + 64) & (n + 32 > jb)).astype(np.float32), np.ones((127, 1), np.float32)], axis=1)
    eblk = (np.arange(S)[None, :] // 64 == np.arange(32)[:, None]).astype(np.float32)
    gsel = np.zeros((48, 24, 128), np.float32)
    for m in range(8):
        for br in range(3):
            gsel[(2 * m) * 3 + br, m * 3 + br, 0:64] = 1.0
            gsel[(2 * m + 1) * 3 + br, m * 3 + br, 64:128] = 1.0
    pos = np.arange(S)
    blk = pos // 64
    jj = np.arange(32)
    forced = (jj[None, :] == 0) | (jj[None, :] == blk[:, None]) | (jj[None, :] == blk[:, None] - 1)
    adm = jj[None, :] * 64 <= pos[:, None]
    seltab = np.zeros((3, S, 64), np.float32)
    for g in range(2):
        seltab[0, :, g * 32:(g + 1) * 32] = forced.astype(np.float32) * 1e6
        seltab[1, :, g * 32:(g + 1) * 32] = adm.astype(np.float32)
        seltab[2, :, g * 32:(g + 1) * 32] = np.where(adm, 0.0, -1e30)
    seltab = seltab.reshape(3, 16, 128, 64)
    shared = {
        'ev_w_in': f(inputs['ev_w_in'][0]), 'ev_ckv_gain': f(inputs['ev_ckv_gain']), 'ev_w_uk': f(inputs['ev_w_uk'][0]),
        'ev_w_uv': f(inputs['ev_w_uv'][0]), 'ev_sinks': f(inputs['ev_sinks']), 'ev_w_out': f(inputs['ev_w_out'][0]),
        'od_w_in': f(inputs['od_w_in'][0]), 'od_pe_k': f(inputs['od_pe_k'][0]), 'od_w1_k': f(inputs['od_w1_k'][0]),
        'od_w2_k': f(inputs['od_w2_k'][0]), 'od_pe_v': f(inputs['od_pe_v'][0]), 'od_w1_v': f(inputs['od_w1_v'][0]),
        'od_w2_v': f(inputs['od_w2_v'][0]), 'od_w_out': f(inputs['od_w_out'][0]),
        'xa_w_q': f(inputs['xa_w_q']), 'xa_w_k': f(inputs['xa_w_k']), 'xa_w_v': f(inputs['xa_w_v']), 'xa_w_o': f(inputs['xa_w_o']),
        'ff_w_gate': f(inputs['ff_w_gate'][0]), 'ff_w_up': f(inputs['ff_w_up'][0]), 'ff_w_down': f(inputs['ff_w_down'][0]),
        'moe_w_router': f(inputs['moe_w_router'][0]), 'moe_w_gate': f(inputs['moe_w_gate'][0]),
        'moe_w_up': f(inputs['moe_w_up'][0]), 'moe_w_down': f(inputs['moe_w_down'][0]),
        'ln_g': f(inputs['ln_g']), 'ln_b': f(inputs['ln_b']), 'bt_tab': bt_tab, 'cfar': cfar,
        'cb_tab': cb_tab, 'ovl': ovl, 'eblk': eblk, 'gsel': gsel, 'seltab': np.ascontiguousarray(seltab),
    }
    return shared


def run(inputs, phases, cores=NCORES):
    key = tuple(phases)
    if key not in _PROG_CACHE:
        _PROG_CACHE[key] = Prog(phases)
    prog = _PROG_CACHE[key]
    shared = make_inputs(inputs)
    x = np.asarray(inputs['x'], dtype=np.float32)
    mem = np.asarray(inputs['mem'], dtype=np.float32)
    in_maps = []
    for c in range(cores):
        m = dict(shared)
        m['x'] = np.ascontiguousarray(x[c * NB:(c + 1) * NB])
        m['mem'] = np.ascontiguousarray(mem[c * NB:(c + 1) * NB])
        in_maps.append(m)
    res = run_bass_kernel_spmd(prog.nc, in_maps, core_ids=list(range(cores)))
    return np.concatenate([r['out'] for r in res.results], axis=0)


def kernel(**inputs):
    return run(inputs, FULL_PHASES)
```

```python
import math
from contextlib import ExitStack

import numpy as np
import concourse.bass as bass
import concourse.mybir as mybir
from concourse.bass_utils import run_bass_kernel_spmd

F32 = mybir.dt.float32
BF16 = mybir.dt.bfloat16
AF = mybir.ActivationFunctionType
ALU = mybir.AluOpType
AX = mybir.AxisListType

NCORES = 8
NB = 2
S = 2048
NT = 16
D = 1024
KC = 8
ALPHA = 4 ** 0.25
LN_EPS = 1e-5
D_FF = 2816
N_EXP = 8
D_FFE = 3584
NEG = -30000.0
import os
STOP = int(os.environ.get('KSTOP', '0'))
KVAR = int(os.environ.get('KVAR', '0'))
NTM = 256 if (KVAR & 32) else 264


class Sem:
    def __init__(self, nc, name):
        self.h = nc.alloc_semaphore(name)
        self.count = 0


class Sched:
    def __init__(self, nc):
        self.nc = nc
        self.eng = {'pe': nc.tensor, 'act': nc.scalar, 'dve': nc.vector, 'pool': nc.gpsimd, 'sp': nc.sync}
        self.sem = {k: Sem(nc, 's_' + k) for k in self.eng}
        self.waited = {k: {} for k in self.eng}
        self.lastw = {}
        self.readers = {}
        self.dsem = {}
        self.ninst = 0

    def _wait(self, e, deps):
        w = self.waited[e]
        best = {}
        for (s, v) in deps:
            if w.get(s, 0) < v and best.get(s, 0) < v:
                best[s] = v
        for s, v in best.items():
            self.eng[e].wait_ge(s.h, v)
            w[s] = v

    def _deps(self, e, reads, writes):
        deps = []
        mine = self.sem.get(e)
        for k in reads:
            t = self.lastw.get(k)
            if t is not None:
                deps.append(t)
            if k.startswith('ps') or k == 'pt':
                deps.extend(r for r in self.readers.get(k, ()) if r[0] is not mine)
        for k in writes:
            t = self.lastw.get(k)
            if t is not None:
                deps.append(t)
            deps.extend(self.readers.get(k, ()))
        if e == 'pe':
            pes = self.sem['pe']
            deps = [d for d in deps if d[0] is not pes]
        return deps

    def _commit(self, tok, reads, writes):
        for k in writes:
            self.lastw[k] = tok
            self.readers[k] = []
        for k in reads:
            if k in writes:
                continue
            r = self.readers.setdefault(k, [])
            r[:] = [t for t in r if t[0] is not tok[0]]
            r.append(tok)

    def op(self, e, fn, reads=(), writes=()):
        self._wait(e, self._deps(e, reads, writes))
        inst = fn(self.eng[e])
        s = self.sem[e]
        s.count += 1
        inst.then_inc(s.h, 1)
        self._commit((s, s.count), reads, writes)
        self.ninst += 1

    def dma(self, e, out, in_, reads=(), writes=(), slot=None):
        self._wait(e, self._deps(e, reads, writes))
        if slot is None:
            slot = writes[0]
        if slot not in self.dsem:
            self.dsem[slot] = Sem(self.nc, 'd%d' % len(self.dsem))
        s = self.dsem[slot]
        inst = self.eng[e].dma_start(out=out, in_=in_)
        s.count += 16
        inst.then_inc(s.h, 16)
        self._commit((s, s.count), reads, writes)
        self.ninst += 1

    def barrier(self):
        allsem = list(self.sem.values()) + list(self.dsem.values())
        for e in self.eng:
            self._wait(e, [(s, s.count) for s in allsem if s.count > 0])
        self.lastw.clear()
        self.readers.clear()


def _rel_bucket_np(dist):
    n = np.maximum(dist, 0)
    nf = np.maximum(n, 1).astype(np.float32)
    log_b = 16 + (np.log(nf / np.float32(16)) / np.float32(math.log(8.0)) * np.float32(16)).astype(np.int32)
    return np.where(n < 16, n, np.minimum(log_b, 31)).astype(np.int64)


class Prog:
    def __init__(self, phases, dbg=False):
        self.phases = phases
        self.dbg = dbg
        nc = bass.Bass("TRN2", target_bir_lowering=False)
        self.nc = nc
        self.S = Sched(nc)
        self.t = {}
        self.declare_io()
        self.build()

    def din(self, name, shape):
        self.t[name] = self.nc.dram_tensor(name, list(shape), F32, kind="ExternalInput").ap()
        return self.t[name]

    def declare_io(self):
        nc = self.nc
        self.din('x', [NB, S, D])
        self.din('mem', [NB, 256, D])
        self.din('ev_w_in', [D, 1992])
        self.din('ev_ckv_gain', [1, 128])
        self.din('ev_w_uk', [8, 64, 128])
        self.din('ev_w_uv', [8, 128, 64])
        self.din('ev_sinks', [1, 8])
        self.din('ev_w_out', [D, D])
        self.din('od_w_in', [D, 1840])
        self.din('od_pe_k', [32, 64])
        self.din('od_w1_k', [2048, 128])
        self.din('od_w2_k', [128, 64])
        self.din('od_pe_v', [32, 64])
        self.din('od_w1_v', [2048, 128])
        self.din('od_w2_v', [128, 64])
        self.din('od_w_out', [D, D])
        self.din('xa_w_q', [2, D, 512])
        self.din('xa_w_k', [2, D, 512])
        self.din('xa_w_v', [2, D, 512])
        self.din('xa_w_o', [2, 512, D])
        self.din('ff_w_gate', [D, D_FF])
        self.din('ff_w_up', [D, D_FF])
        self.din('ff_w_down', [D_FF, D])
        self.din('moe_w_router', [D, N_EXP])
        self.din('moe_w_gate', [N_EXP, D, D_FFE])
        self.din('moe_w_up', [N_EXP, D, D_FFE])
        self.din('moe_w_down', [N_EXP, D_FFE, D])
        self.din('ln_g', [2, 3, D])
        self.din('ln_b', [2, 3, D])
        self.din('bt_tab', [16, 128, 256])
        self.din('cfar', [1, 16])
        self.din('cb_tab', [16, 4, 127, 512])
        self.din('ovl', [127, 33])
        self.din('eblk', [32, S])
        self.din('gsel', [48, 24, 128])
        self.din('seltab', [3, 16, 128, 64])
        self.out = nc.dram_tensor("out", [NB, S, D], F32, kind="ExternalOutput").ap()
        self.hscr = nc.dram_tensor("hscr", [NB, S, D], F32, kind="Internal").ap()

    def sb(self, st, name, shape, dt):
        self.uid = getattr(self, 'uid', 0) + 1
        return st.enter_context(self.nc.sbuf_tensor('%s_u%d' % (name, self.uid), list(shape), dt))

    def bcast_rows(self, ap_row, n):
        return bass.AP(ap_row.tensor, ap_row.offset, [[0, 128], [1, n]])

    def build(self):
        nc, S_ = self.nc, self.S
        with ExitStack() as gst:
            self.ident = self.sb(gst, 'ident', [128, 128], BF16)
            self.ones = self.sb(gst, 'ones', [128, 128], BF16)
            self.hT = self.sb(gst, 'hT', [128, KC, S], BF16)
            self.ps = [gst.enter_context(nc.psum_tensor('ps%d' % i, [128, 512], F32)) for i in range(7)]
            self.pt = gst.enter_context(nc.psum_tensor('pt', [128, 1024], BF16))
            S_.op('pool', lambda e: e.memset(self.ident[:], 1.0), writes=['ident'])
            S_.op('pool', lambda e: e.affine_select(out=self.ident[:], in_=self.ident[:], pattern=[[-1, 128]],
                                                      compare_op=ALU.is_equal, fill=0.0, base=0, channel_multiplier=1),
                  reads=['ident'], writes=['ident'])
            S_.op('pool', lambda e: e.memset(self.ones[:], 1.0), writes=['ones'])
            self.eps_t = self.sb(gst, 'eps_t', [128, 2], F32)
            S_.op('pool', lambda e: e.memset(self.eps_t[:], LN_EPS), writes=['eps_t'])
            S_.barrier()
            for b in range(NB):
                cur = ('x', b)
                for ph in self.phases:
                    if ph == 'x0':
                        self.phase_x0(b)
                    elif ph.startswith('xattn'):
                        i = int(ph[-1])
                        last = (ph == self.phases[-1])
                        self.phase_xattn(b, i, cur, ('out', b) if last else ('hscr', b))
                        cur = ('hscr', b)
                    elif ph == 'ffn':
                        last = (ph == self.phases[-1])
                        self.phase_ffn(b, cur, ('out', b) if last else ('hscr', b), moe=False)
                        cur = ('hscr', b)
                    elif ph == 'moe':
                        last = (ph == self.phases[-1])
                        self.phase_ffn(b, cur, ('out', b) if last else ('hscr', b), moe=True)
                        cur = ('hscr', b)
                    elif ph == 'mix0':
                        last = (ph == self.phases[-1])
                        self.phase_mix0(b, cur, ('out', b) if last else ('hscr', b))
                        cur = ('hscr', b)
                    elif ph == 'mix1':
                        last = (ph == self.phases[-1])
                        self.phase_mix1(b, cur, ('out', b) if last else ('hscr', b))
                        cur = ('hscr', b)
                    else:
                        raise ValueError(ph)
                    S_.barrier()
            S_.barrier()

    def dram_tile(self, loc, t):
        kind, b = loc
        base = {'x': self.t['x'], 'hscr': self.hscr, 'out': self.out}[kind]
        return base[b, t * 128:(t + 1) * 128, :]

    def to_hT(self, src_bf, src_key, t, eng='dve'):
        S_ = self.S
        for kc in range(KC):
            S_.op('pe', lambda e: e.transpose(self.pt[:, kc * 128:(kc + 1) * 128], src_bf[:, kc * 128:(kc + 1) * 128],
                                              self.ident[:]),
                  reads=[src_key, 'ident'], writes=['pt'])
        dst = self.hT[:, :, t * 128:(t + 1) * 128]
        src = self.pt[:].rearrange("p (k n) -> p k n", k=KC)
        hk = 'hT%d' % (t // 4)
        if eng == 'dve':
            S_.op('dve', lambda e: e.tensor_copy(out=dst, in_=src), reads=['pt'], writes=[hk])
        else:
            S_.op('act', lambda e: e.activation(out=dst, in_=src, func=AF.Copy), reads=['pt'], writes=[hk])


    def run_blocks(self, blocks):
        n = len(blocks)
        for i in range(n + 1):
            if i < n:
                sj = 2 + (i % 2)
                blocks[i]['qk'](self.ps[sj], 'ps%d' % sj, i % 2)
            if i >= 1:
                b_ = blocks[i - 1]
                b_['pv']((i - 1) % 2)
                if b_.get('post'):
                    b_['post']()

    def phase_x0(self, b):
        S_ = self.S
        with ExitStack() as st:
            xb = [self.sb(st, 'x0b%d' % i, [128, D], BF16) for i in range(2)]
            for t in range(NT):
                k = 'x0b%d' % (t % 2)
                S_.dma('pool', xb[t % 2][:], self.t['x'][b, t * 128:(t + 1) * 128, :], writes=[k])
                self.to_hT(xb[t % 2], k, t, eng='dve' if t % 2 == 0 else 'act')
            self.S.barrier()

    def ln_setup(self, st, i, k, alias=None):
        S_ = self.S
        self.ln_g = self.sb(st, 'ln_g', [128, D], F32)
        self.ln_b = self.sb(st, 'ln_b', [128, D], F32)
        S_.dma('sp', self.ln_g[:], self.bcast_rows(self.t['ln_g'][i, k:k + 1, :], D), writes=['ln_g'])
        S_.dma('sp', self.ln_b[:], self.bcast_rows(self.t['ln_b'][i, k:k + 1, :], D), writes=['ln_b'])
        if alias is None:
            self.ln_res = [self.sb(st, 'ln_res0', [128, D], F32)] * 2
            self.ln_t = [self.sb(st, 'ln_t0', [128, D], F32)] * 2
            self.ln_keys = ('ln_res0', 'ln_t0')
        else:
            buf, key = alias
            self.ln_res = [buf[:, 0:D]] * 2
            self.ln_t = [buf[:, D:2 * D]] * 2
            self.ln_keys = (key, key)
        self.ln_hb = [self.sb(st, 'ln_hb0', [128, D], BF16)] * 2
        self.ln_st = [self.sb(st, 'ln_st%d' % j, [128, 16], F32) for j in range(2)]
        self.ln_cnt = 0

    def ln_tile(self, srcs, src_keys, t, res_loc, out_loc, want_hT=True):
        S_ = self.S
        j = self.ln_cnt % 2
        self.ln_cnt += 1
        res, tt, hb, stt = self.ln_res[j], self.ln_t[j], self.ln_hb[j], self.ln_st[j]
        kres, kt, khb, kst = self.ln_keys[0], self.ln_keys[1], 'ln_hb0', 'ln_st%d' % j
        S_.dma('sp', res[:], self.dram_tile(res_loc, t), reads=['dram_%s_%d' % (res_loc[0], t)], writes=[kres])
        for hf in range(2):
            sl = slice(hf * 512, (hf + 1) * 512)
            S_.op('dve', lambda e: e.scalar_tensor_tensor(out=tt[:, sl], in0=res[:, sl], scalar=ALPHA, in1=srcs[hf],
                                                          op0=ALU.mult, op1=ALU.add),
                  reads=[kres, src_keys[hf]], writes=[kt])
        for hf in range(2):
            sl = slice(hf * 512, (hf + 1) * 512)
            S_.op('dve', lambda e: e.bn_stats(out=stt[:, hf * 6:(hf + 1) * 6], in_=tt[:, sl]), reads=[kt], writes=[kst])
        S_.op('dve', lambda e: e.bn_aggr(out=stt[:, 12:14], in_=stt[:, 0:12]),
              reads=[kst], writes=[kst])
        S_.op('act', lambda e: e.activation(out=stt[:, 14:15], in_=stt[:, 13:14], func=AF.Ln, bias=self.eps_t[:, 0:1], scale=1.0),
              reads=[kst, 'eps_t'], writes=[kst])
        S_.op('act', lambda e: e.activation(out=stt[:, 14:15], in_=stt[:, 14:15], func=AF.Exp, scale=-0.5), reads=[kst], writes=[kst])
        S_.op('dve', lambda e: e.scalar_tensor_tensor(out=stt[:, 15:16], in0=stt[:, 12:13], scalar=-1.0, in1=stt[:, 14:15],
                                                      op0=ALU.mult, op1=ALU.mult), reads=[kst], writes=[kst])
        S_.op('act', lambda e: e.activation(out=tt[:], in_=tt[:], func=AF.Identity, bias=stt[:, 15:16], scale=stt[:, 14:15]),
              reads=[kt, kst], writes=[kt])
        S_.op('pool', lambda e: e.tensor_tensor(out=tt[:], in0=tt[:], in1=self.ln_g[:], op=ALU.mult),
              reads=[kt, 'ln_g'], writes=[kt])
        S_.op('pool', lambda e: e.tensor_tensor(out=tt[:], in0=tt[:], in1=self.ln_b[:], op=ALU.add),
              reads=[kt, 'ln_b'], writes=[kt])
        S_.dma('sp', self.dram_tile(out_loc, t), tt[:], reads=[kt], writes=['dram_%s_%d' % (out_loc[0], t)],
               slot='st_%d' % j)
        if want_hT:
            S_.op('act', lambda e: e.activation(out=hb[:], in_=tt[:], func=AF.Copy), reads=[kt], writes=[khb])
            self.to_hT(hb, khb, t, eng='dve')

    def load_w(self, dst, src, key):
        self.S.dma('pool', dst, src, writes=[key])

    def phase_xattn(self, b, i, res_loc, out_loc):
        S_, ps = self.S, self.ps
        sc = 128 ** -0.5
        with ExitStack() as st:
            wq = self.sb(st, 'xwq', [128, KC, 512], BF16)
            wk = self.sb(st, 'xwk', [128, KC, 512], BF16)
            wv = self.sb(st, 'xwv', [128, KC, 512], BF16)
            wo = self.sb(st, 'xwo', [128, 4, D], BF16)
            memb = self.sb(st, 'memb', [128, 2, D], BF16)
            memT = self.sb(st, 'memT', [128, KC, 256], BF16)
            kxT = self.sb(st, 'kxT', [128, 4, 256], BF16)
            vx = self.sb(st, 'vx', [128, 2, 512], BF16)
            qxT = self.sb(st, 'qxT', [128, 4, 512], BF16)
            xoT = self.sb(st, 'xoT', [128, 4, 512], BF16)
            pT = [self.sb(st, 'xpT%d' % j, [128, 512], BF16) for j in range(2)]
            rd = self.sb(st, 'xrd', [128, 512], F32)
            self.ln_setup(st, i, 1)
            self.load_w(wk[:], self.t['xa_w_k'][i].rearrange("(k p) n -> p k n", p=128), 'xwk')
            self.load_w(wv[:], self.t['xa_w_v'][i].rearrange("(k p) n -> p k n", p=128), 'xwv')
            self.load_w(memb[:], self.t['mem'][b].rearrange("(t p) d -> p t d", p=128), 'memb')
            self.load_w(wq[:], self.t['xa_w_q'][i].rearrange("(k p) n -> p k n", p=128), 'xwq')
            self.load_w(wo[:], self.t['xa_w_o'][i].rearrange("(k p) n -> p k n", p=128), 'xwo')
            for mt in range(2):
                for kc in range(KC):
                    S_.op('pe', lambda e: e.transpose(self.pt[:, kc * 128:(kc + 1) * 128],
                                                      memb[:, mt, kc * 128:(kc + 1) * 128], self.ident[:]),
                          reads=['memb', 'ident'], writes=['pt'])
                S_.op('dve', lambda e: e.tensor_copy(out=memT[:, :, mt * 128:(mt + 1) * 128],
                                                     in_=self.pt[:].rearrange("p (k n) -> p k n", k=KC)),
                      reads=['pt'], writes=['memT'])
            for h in range(4):
                for kc in range(KC):
                    S_.op('pe', lambda e: e.matmul(ps[0][:, 0:256], wk[:, kc, h * 128:(h + 1) * 128], memT[:, kc, :],
                                                   start=(kc == 0), stop=(kc == KC - 1)),
                          reads=['xwk', 'memT'], writes=['ps0'])
                S_.op('act', lambda e: e.activation(out=kxT[:, h, :], in_=ps[0][:, 0:256], func=AF.Copy),
                      reads=['ps0'], writes=['kxT'])
            for mt in range(2):
                for kc in range(KC):
                    S_.op('pe', lambda e: e.matmul(ps[1][:], memT[:, kc, mt * 128:(mt + 1) * 128], wv[:, kc, :],
                                                   start=(kc == 0), stop=(kc == KC - 1)),
                          reads=['xwv', 'memT'], writes=['ps1'])
                S_.op('dve', lambda e: e.tensor_copy(out=vx[:, mt, :], in_=ps[1][:]), reads=['ps1'], writes=['vx'])
            for c in range(4):
                hk = 'hT%d' % c
                for h in range(4):
                    for kc in range(KC):
                        S_.op('pe', lambda e: e.matmul(ps[h % 2][:], wq[:, kc, h * 128:(h + 1) * 128],
                                                       self.hT[:, kc, c * 512:(c + 1) * 512],
                                                       start=(kc == 0), stop=(kc == KC - 1)),
                              reads=['xwq', hk], writes=['ps%d' % (h % 2)])
                    if h % 2 == 0:
                        S_.op('act', lambda e: e.activation(out=qxT[:, h, :], in_=ps[h % 2][:], func=AF.Copy),
                              reads=['ps%d' % (h % 2)], writes=['qxT%d' % h])
                    else:
                        S_.op('dve', lambda e: e.tensor_copy(out=qxT[:, h, :], in_=ps[h % 2][:]),
                              reads=['ps%d' % (h % 2)], writes=['qxT%d' % h])
                blocks = []
                for h in range(4):
                    for mt in range(2):
                        def qk(sT, sk, pj, h=h, mt=mt):
                            S_.op('pe', lambda e: e.matmul(sT[:], kxT[:, h, mt * 128:(mt + 1) * 128], qxT[:, h, :],
                                                           start=True, stop=True), reads=['kxT', 'qxT%d' % h], writes=[sk])
                            S_.op('act', lambda e: e.activation(out=pT[pj][:], in_=sT[:], func=AF.Exp, scale=sc),
                                  reads=[sk], writes=['xpT%d' % pj])

                        def pv(pj, h=h, mt=mt):
                            S_.op('pe', lambda e: e.matmul(ps[4][:], vx[:, mt, h * 128:(h + 1) * 128], pT[pj][:],
                                                           start=(mt == 0), stop=(mt == 1)), reads=['vx', 'xpT%d' % pj], writes=['ps4'])
                            S_.op('pe', lambda e: e.matmul(ps[5][:], self.ones[:], pT[pj][:],
                                                           start=(mt == 0), stop=(mt == 1)), reads=['ones', 'xpT%d' % pj], writes=['ps5'])

                        def post(h=h):
                            S_.op('act', lambda e: e.activation(out=rd[:], in_=ps[5][:], func=AF.Ln), reads=['ps5'], writes=['xrd'])
                            S_.op('act', lambda e: e.activation(out=rd[:], in_=rd[:], func=AF.Exp, scale=-1.0), reads=['xrd'], writes=['xrd'])
                            S_.op('dve', lambda e: e.tensor_tensor(out=xoT[:, h, :], in0=rd[:], in1=ps[4][:], op=ALU.mult),
                                  reads=['xrd', 'ps4'], writes=['xoT'])
                        blocks.append({'qk': qk, 'pv': pv, 'post': post if mt == 1 else None})
                self.run_blocks(blocks)
                for tt in range(4):
                    for hf in range(2):
                        for h in range(4):
                            S_.op('pe', lambda e: e.matmul(ps[hf][:], xoT[:, h, tt * 128:(tt + 1) * 128],
                                                           wo[:, h, hf * 512:(hf + 1) * 512],
                                                           start=(h == 0), stop=(h == 3)),
                                  reads=['xoT', 'xwo'], writes=['ps%d' % hf])
                    self.ln_tile([ps[0][:], ps[1][:]], ['ps0', 'ps1'], c * 4 + tt, res_loc, out_loc)
            S_.barrier()

    def phase_mix0(self, b, res_loc, out_loc):
        S_, ps, nc = self.S, self.ps, self.nc
        W = self.t['ev_w_in']
        NIT = 14
        with ExitStack() as st:
            wqa = self.sb(st, 'wqa', [128, KC, 512], BF16)
            wqi = self.sb(st, 'wqi', [128, KC, 512], BF16)
            wqb = self.sb(st, 'wqb', [128, KC, 512], BF16)
            wuk = self.sb(st, 'wuk', [128, 4, 128], BF16)
            wout = self.sb(st, 'wout', [128, KC, D], BF16)
            cT = self.sb(st, 'cT', [128, S], BF16)
            vA = self.sb(st, 'vA', [128, NT, 8, 128], BF16)
            kiT2 = self.sb(st, 'kiT2', [128, S], BF16)
            kbT = self.sb(st, 'kbT', [128, S], BF16)
            vb = self.sb(st, 'vb', [128, NT, 2, 128], BF16)
            absw = self.sb(st, 'absw', [128, NT, 8], F32)
            sgnw = self.sb(st, 'sgnw', [128, NT, 8], F32)
            TB = self.sb(st, 'TB', [128, 16, 256], BF16)
            cfar = self.sb(st, 'cfar', [128, 16], F32)
            esink = self.sb(st, 'esink', [128, 8], F32)
            tri = self.sb(st, 'tri', [128, 128], F32)
            pw = self.sb(st, 'pw', [128, NIT], F32)
            self.load_w(wqa[:], W[:, 0:512].rearrange("(k p) n -> p k n", p=128), 'wqa')
            self.load_w(wqi[:], W[:, 640:1152].rearrange("(k p) n -> p k n", p=128), 'wqi')
            for hf in range(2):
                for j in range(4):
                    c0 = 1224 + (hf * 4 + j) * 64
                    self.load_w(wqb[:, :, j * 128 + hf * 64:j * 128 + (hf + 1) * 64],
                                W[:, c0:c0 + 64].rearrange("(k p) n -> p k n", p=128), 'wqb')
            for hh in range(2):
                self.load_w(wuk[hh * 64:(hh + 1) * 64, :, :],
                            self.t['ev_w_uk'].rearrange("(j f) d c -> f d j c", f=2)[hh], 'wuk')
            self.load_w(wout[:], self.t['ev_w_out'].rearrange("(k p) n -> p k n", p=128), 'wout')
            S_.dma('sp', cfar[:], self.bcast_rows(self.t['cfar'][0:1, :], 16), writes=['cfar'])
            S_.dma('sp', esink[:], self.bcast_rows(self.t['ev_sinks'][0:1, :], 8), writes=['esink'])
            S_.op('act', lambda e: e.activation(out=esink[:], in_=esink[:], func=AF.Exp), reads=['esink'], writes=['esink'])
            esink2 = self.sb(st, 'esink2', [128, 4], F32)
            ev = esink[:].rearrange("p (m f) -> p m f", f=2)
            S_.op('dve', lambda e: e.tensor_copy(out=esink2[0:64, :], in_=ev[0:64, :, 0]), reads=['esink'], writes=['esink2'])
            S_.op('dve', lambda e: e.tensor_copy(out=esink2[64:128, :], in_=ev[64:128, :, 1]), reads=['esink'], writes=['esink2'])
            S_.op('pool', lambda e: e.memset(tri[:], 0.0), writes=['tri'])
            S_.op('pool', lambda e: e.memset(vb[:], 1.0), writes=['vb'])
            S_.op('pool', lambda e: e.memset(vA[:], 1.0), writes=['vA'])
            S_.op('pool', lambda e: e.affine_select(out=tri[:], in_=tri[:], pattern=[[-1, 128]], compare_op=ALU.is_ge,
                                                      fill=-1e30, base=0, channel_multiplier=1), reads=['tri'], writes=['tri'])
            for i in range(NIT):
                S_.op('pool', lambda e: e.memset(pw[:, i:i + 1], 2.0 ** (-i)), writes=['pw'])
            with ExitStack() as st2:
                bt = [self.sb(st2, 'bt%d' % i, [128, 256], F32) for i in range(2)]
                swam = self.sb(st2, 'swam', [128, 256], F32)
                S_.op('pool', lambda e: e.memset(swam[:], 0.0), writes=['swam'])
                S_.op('pool', lambda e: e.affine_select(out=swam[:], in_=swam[:], pattern=[[1, 256]], compare_op=ALU.is_ge,
                                                          fill=NEG, base=0, channel_multiplier=-1), reads=['swam'], writes=['swam'])
                S_.op('pool', lambda e: e.affine_select(out=swam[:], in_=swam[:], pattern=[[-1, 256]], compare_op=ALU.is_ge,
                                                          fill=NEG, base=127, channel_multiplier=1), reads=['swam'], writes=['swam'])
                for h in range(16):
                    k = 'bt%d' % (h % 2)
                    S_.dma('sp', bt[h % 2][:], self.t['bt_tab'][h], writes=[k])
                    if h < 8:
                        S_.op('dve', lambda e: e.tensor_scalar(out=TB[:, h, :], in0=bt[h % 2][:], scalar1=cfar[:, h:h + 1],
                                                               scalar2=8.0, op0=ALU.subtract, op1=ALU.mult),
                              reads=[k, 'cfar'], writes=['TB'])
                    else:
                        S_.op('dve', lambda e: e.scalar_tensor_tensor(out=TB[:, h, :], in0=bt[h % 2][:], scalar=8.0, in1=swam[:],
                                                                      op0=ALU.mult, op1=ALU.add),
                              reads=[k, 'swam'], writes=['TB'])
                S_.barrier()
            if STOP == 1:
                return
            with ExitStack() as st2:
                wtm = self.sb(st2, 'wtm', [128, KC, 288], BF16)
                wki2 = self.sb(st2, 'wki2', [128, KC, 128], BF16)
                wkb = self.sb(st2, 'wkb', [128, KC, 128], BF16)
                wuv = self.sb(st2, 'wuv', [128, 8, 64], BF16)
                gain = self.sb(st2, 'gain', [128, 128], F32)
                cb = [self.sb(st2, 'cb%d' % i, [128, 128], BF16) for i in range(2)]
                sq = self.sb(st2, 'sq', [128, 128], F32)
                ss = [self.sb(st2, 'ss%d' % i, [128, 4], F32) for i in range(2)]
                r3 = lambda a: a.rearrange("(k p) n -> p k n", p=128)
                self.load_w(wtm[:, :, 0:128], r3(W[:, 512:640]), 'wtm')
                self.load_w(wtm[:, :, 128:256], r3(W[:, 1864:1992]), 'wtm')
                if not (KVAR & 1):
                    self.load_w(wtm[:, :, 256:264], r3(W[:, 1216:1224]), 'wtm')
                else:
                    S_.op('pool', lambda e: e.memset(wtm[:, :, 256:264], 0.5), writes=['wtm'])
                self.load_w(wki2[:, :, 0:64], r3(W[:, 1152:1216]), 'wki2')
                self.load_w(wki2[:, :, 64:128], r3(W[:, 1152:1216]), 'wki2')
                self.load_w(wkb[:], r3(W[:, 1736:1864]), 'wkb')
                self.load_w(wuv[:], self.t['ev_w_uv'].rearrange("h c d -> c h d"), 'wuv')
                S_.dma('sp', gain[:], self.bcast_rows(self.t['ev_ckv_gain'][0:1, :], 128), writes=['gain'])
                if STOP == 21:
                    S_.barrier()
                    return
                for t in range(NT if STOP != 22 else 1):
                    hk = 'hT%d' % (t // 4)
                    i2 = t % 2
                    for kc in range(KC):
                        S_.op('pe', lambda e: e.matmul(ps[0][:, 0:NTM], self.hT[:, kc, t * 128:(t + 1) * 128], wtm[:, kc, 0:NTM],
                                                       start=(kc == 0), stop=(kc == KC - 1)), reads=[hk, 'wtm'], writes=['ps0'])
                    if not (KVAR & 4):
                        S_.op('act', lambda e: e.activation(out=sq[:], in_=ps[0][:, 0:128], func=AF.Square,
                                                            accum_out=ss[i2][:, 0:1]), reads=['ps0'], writes=['sq', 'ss%d' % i2])
                    if not (KVAR & 4):
                        S_.op('act', lambda e: e.activation(out=ss[i2][:, 1:2], in_=ss[i2][:, 0:1], func=AF.Ln, bias=self.eps_t[:, 0:1],
                                                            scale=1.0 / 128), reads=['ss%d' % i2, 'eps_t'], writes=['ss%d' % i2])
                    if not (KVAR & 4):
                        S_.op('act', lambda e: e.activation(out=ss[i2][:, 2:3], in_=ss[i2][:, 1:2], func=AF.Exp, scale=-0.5),
                              reads=['ss%d' % i2], writes=['ss%d' % i2])
                    if not (KVAR & 64):
                        S_.op('dve', lambda e: e.scalar_tensor_tensor(out=cb[i2][:], in0=ps[0][:, 0:128], scalar=ss[i2][:, 2:3],
                                                                      in1=gain[:], op0=ALU.mult, op1=ALU.mult),
                              reads=['ps0', 'ss%d' % i2, 'gain'], writes=['cb%d' % i2])
                    if not (KVAR & 128):
                        S_.op('act', lambda e: e.activation(out=vb[:, t, :, 0:64], in_=ps[0][:, 128:256].rearrange("p (g d) -> p g d", g=2),
                                                            func=AF.Copy), reads=['ps0'], writes=['vb'])
                    if not (KVAR & 2):
                        S_.op('act', lambda e: e.activation(out=absw[:, t, :], in_=ps[0][:, 256:264], func=AF.Abs),
                              reads=['ps0'], writes=['absw'])
                    if not (KVAR & 2):
                        S_.op('act', lambda e: e.activation(out=sgnw[:, t, :], in_=ps[0][:, 256:264], func=AF.Sign),
                              reads=['ps0'], writes=['sgnw'])
                    if not (KVAR & 8):
                        S_.op('pe', lambda e: e.transpose(self.pt[:, 0:128], cb[i2][:], self.ident[:]),
                              reads=['cb%d' % i2, 'ident'], writes=['pt'])
                    if not (KVAR & 8):
                        S_.op('act', lambda e: e.activation(out=cT[:, t * 128:(t + 1) * 128], in_=self.pt[:, 0:128], func=AF.Copy),
                              reads=['pt'], writes=['cT'])
                    if not (KVAR & 8):
                        S_.op('pe', lambda e: e.matmul(ps[1][:], cT[:, t * 128:(t + 1) * 128], wuv[:].rearrange("p h d -> p (h d)"),
                                                       start=True, stop=True), reads=['cT', 'wuv'], writes=['ps1'])
                    if not (KVAR & 8):
                        S_.op('dve', lambda e: e.tensor_copy(out=vA[:, t, :, 0:64], in_=ps[1][:].rearrange("p (h d) -> p h d", h=8)), reads=['ps1'], writes=['vA'])
                for cc in range(4 if not (KVAR & 16) else 0):
                    hk = 'hT%d' % cc
                    sl = slice(cc * 512, (cc + 1) * 512)
                    for kc in range(KC):
                        S_.op('pe', lambda e: e.matmul(ps[2][:], wki2[:, kc, :], self.hT[:, kc, sl],
                                                       start=(kc == 0), stop=(kc == KC - 1)), reads=[hk, 'wki2'], writes=['ps2'])
                    S_.op('act', lambda e: e.activation(out=kiT2[:, sl], in_=ps[2][:], func=AF.Copy), reads=['ps2'], writes=['kiT2'])
                    for kc in range(KC):
                        S_.op('pe', lambda e: e.matmul(ps[3][:], wkb[:, kc, :], self.hT[:, kc, sl],
                                                       start=(kc == 0), stop=(kc == KC - 1)), reads=[hk, 'wkb'], writes=['ps3'])
                    S_.op('dve', lambda e: e.tensor_copy(out=kbT[:, sl], in_=ps[3][:]), reads=['ps3'], writes=['kbT'])
                S_.barrier()
            if STOP == 2:
                return
            qlatT = self.sb(st, 'qlatT', [128, 8, 512], BF16)
            qa_t = [self.sb(st, 'qa_t0', [128, 512], BF16)] * 2
            qiT = self.sb(st, 'qiT', [128, 4, 512], BF16)
            qbT = self.sb(st, 'qbT', [128, 4, 512], BF16)
            score = self.sb(st, 'score', [128, S], F32)
            maskb = self.sb(st, 'maskb', [128, S], BF16)
            rtmp = [self.sb(st, 'rtmp%d' % i, [128, 512], F32) for i in range(2)]
            nm_off = self.sb(st, 'nm_off', [128, 12, 512], BF16)
            nm_dg = self.sb(st, 'nm_dg', [128, 4, 512], BF16)
            pT = [self.sb(st, 'pT%d' % i, [128, 512], BF16) for i in range(2)]
            rd = self.sb(st, 'rd', [128, 512], F32)
            aoT = self.sb(st, 'aoT', [128, KC, 512], BF16)
            bs = self.sb(st, 'bs', [128, 8], F32)
            Dk = self.sb(st, 'Dk', [128, NIT], F32)
            self.ln_setup(st, 0, 0, alias=(score, 'score'))
            S_.op('pool', lambda e: e.memset(nm_dg[:], -1024.0), writes=['nm_dg'])
            sc = 0.125
            pcnt = 0
            for c in range(4):
                hk = 'hT%d' % c
                qsl = slice(c * 512, (c + 1) * 512)
                for j in range(4):
                    qt = qa_t[j % 2]
                    qk = 'qa_t0'
                    for kc in range(KC):
                        S_.op('pe', lambda e: e.matmul(ps[0][:], wqa[:, kc, j * 128:(j + 1) * 128], self.hT[:, kc, qsl],
                                                       start=(kc == 0), stop=(kc == KC - 1)), reads=[hk, 'wqa'], writes=['ps0'])
                    S_.op('act', lambda e: e.activation(out=qt[:], in_=ps[0][:], func=AF.Copy), reads=['ps0'], writes=[qk])
                    for hh in range(2):
                        h = 2 * j + hh
                        rows = slice(hh * 64, (hh + 1) * 64)
                        S_.op('pe', lambda e: e.matmul(ps[1][:], wuk[rows, j, :], qt[rows, :], start=True, stop=True),
                              reads=[qk, 'wuk'], writes=['ps1'])
                        S_.op('dve', lambda e: e.tensor_copy(out=qlatT[:, h, :], in_=ps[1][:]), reads=['ps1'], writes=['qlatT'])
                    for kc in range(KC):
                        S_.op('pe', lambda e: e.matmul(ps[0][:], wqi[:, kc, j * 128:(j + 1) * 128], self.hT[:, kc, qsl],
                                                       start=(kc == 0), stop=(kc == KC - 1)), reads=[hk, 'wqi'], writes=['ps0'])
                    S_.op('act', lambda e: e.activation(out=qiT[:, j, :], in_=ps[0][:], func=AF.Copy), reads=['ps0'], writes=['qiT'])
                    for kc in range(KC):
                        S_.op('pe', lambda e: e.matmul(ps[1][:], wqb[:, kc, j * 128:(j + 1) * 128], self.hT[:, kc, qsl],
                                                       start=(kc == 0), stop=(kc == KC - 1)), reads=[hk, 'wqb'], writes=['ps1'])
                    S_.op('dve', lambda e: e.tensor_copy(out=qbT[:, j, :], in_=ps[1][:]), reads=['ps1'], writes=['qbT'])
                if STOP == 3:
                    S_.barrier()
                    return
                for tt in range(4):
                    tq = 4 * c + tt
                    nk = (tq + 1) * 128
                    for kch in range((nk + 511) // 512):
                        k0 = kch * 512
                        ncol = min(512, nk - k0)
                        for hi in range(8):
                            j, hh = hi // 2, hi % 2
                            rows = slice(hh * 64, (hh + 1) * 64)
                            pj = pcnt % 2
                            pcnt += 1
                            S_.op('pe', lambda e: e.matmul(ps[pj][:, 0:ncol], qiT[rows, j, tt * 128:(tt + 1) * 128],
                                                           kiT2[rows, k0:k0 + ncol], start=True, stop=True),
                                  reads=['qiT', 'kiT2'], writes=['ps%d' % pj])
                            S_.op('act', lambda e: e.activation(out=rtmp[pj][:, 0:ncol], in_=ps[pj][:, 0:ncol], func=AF.Relu,
                                                                scale=absw[:, tq, hi:hi + 1]),
                                  reads=['ps%d' % pj, 'absw'], writes=['rtmp%d' % pj])
                            if hi == 0:
                                S_.op('dve', lambda e: e.tensor_scalar(out=score[:, k0:k0 + ncol], in0=rtmp[pj][:, 0:ncol],
                                                                       scalar1=sgnw[:, tq, 0:1], scalar2=None, op0=ALU.mult),
                                      reads=['rtmp%d' % pj, 'sgnw'], writes=['score'])
                            else:
                                S_.op('dve', lambda e: e.scalar_tensor_tensor(out=score[:, k0:k0 + ncol], in0=rtmp[pj][:, 0:ncol],
                                                                              scalar=sgnw[:, tq, hi:hi + 1],
                                                                              in1=score[:, k0:k0 + ncol], op0=ALU.mult, op1=ALU.add),
                                      reads=['rtmp%d' % pj, 'sgnw', 'score'], writes=['score'])
                    if tq >= 2:
                        S_.op('dve', lambda e: e.tensor_reduce(out=bs[:, 0:1], in_=score[:, 0:nk], axis=AX.X, op=ALU.max,
                                                               apply_absolute_value=True), reads=['score'], writes=['bs'])
                    S_.op('dve', lambda e: e.tensor_tensor(out=score[:, tq * 128:nk], in0=score[:, tq * 128:nk], in1=tri[:],
                                                           op=ALU.add), reads=['score', 'tri'], writes=['score'])
                    if tq >= 2:
                        S_.op('dve', lambda e: e.tensor_scalar(out=Dk[:], in0=pw[:], scalar1=bs[:, 0:1], scalar2=None, op0=ALU.mult),
                              reads=['bs', 'pw'], writes=['Dk'])
                        S_.op('dve', lambda e: e.tensor_scalar(out=bs[:, 1:2], in0=bs[:, 0:1], scalar1=-1.0, scalar2=None,
                                                               op0=ALU.mult), reads=['bs'], writes=['bs'])
                        for it in range(NIT):
                            S_.op('dve', lambda e: e.tensor_tensor(out=bs[:, 2:3], in0=bs[:, 1:2], in1=Dk[:, it:it + 1], op=ALU.add),
                                  reads=['bs', 'Dk'], writes=['bs'])
                            S_.op('dve', lambda e: e.tensor_scalar(out=maskb[:, 0:nk], in0=score[:, 0:nk], scalar1=bs[:, 2:3],
                                                                   scalar2=0.0, op0=ALU.is_ge, op1=ALU.add, accum_out=bs[:, 3:4]),
                                  reads=['score', 'bs'], writes=['maskb', 'bs'])
                            S_.op('dve', lambda e: e.scalar_tensor_tensor(out=bs[:, 4:5], in0=bs[:, 3:4], scalar=255.5,
                                                                          in1=Dk[:, it:it + 1], op0=ALU.is_ge, op1=ALU.mult),
                                  reads=['bs', 'Dk'], writes=['bs'])
                            S_.op('dve', lambda e: e.tensor_tensor(out=bs[:, 1:2], in0=bs[:, 1:2], in1=bs[:, 4:5], op=ALU.add),
                                  reads=['bs'], writes=['bs'])
                    else:
                        S_.op('dve', lambda e: e.memset(bs[:, 1:2], -1e29), writes=['bs'])
                    S_.op('dve', lambda e: e.tensor_scalar(out=maskb[:, 0:nk], in0=score[:, 0:nk], scalar1=bs[:, 1:2],
                                                           scalar2=-1024.0, op0=ALU.is_lt, op1=ALU.mult),
                          reads=['score', 'bs'], writes=['maskb'])
                    kt = 0
                    while kt <= tq:
                        lim = 4 * c if kt < 4 * c else tq + 1
                        n = min(8, lim - kt)
                        for i in range(n):
                            S_.op('pe', lambda e: e.transpose(self.pt[:, i * 128:(i + 1) * 128],
                                                              maskb[:, (kt + i) * 128:(kt + i + 1) * 128], self.ident[:]),
                                  reads=['maskb', 'ident'], writes=['pt'])
                        src = self.pt[:, 0:n * 128].rearrange("p (k n) -> p k n", k=n)
                        if kt < 4 * c:
                            dst = nm_off[:, kt:kt + n, tt * 128:(tt + 1) * 128]
                            S_.op('act', lambda e: e.activation(out=dst, in_=src, func=AF.Copy), reads=['pt'], writes=['nm_off'])
                        else:
                            dst = nm_dg[:, kt - 4 * c:kt - 4 * c + n, tt * 128:(tt + 1) * 128]
                            S_.op('act', lambda e: e.activation(out=dst, in_=src, func=AF.Copy), reads=['pt'], writes=['nm_dg'])
                        kt += n
                if STOP == 4:
                    S_.barrier()
                    return
                nkt = 4 * c + 4
                blocks = []
                for h in range(8):
                    j, hh = h // 2, h % 2
                    rows = slice(hh * 64, (hh + 1) * 64)
                    for kt in range(nkt):
                        def qk(sT, sk, pj, h=h, kt=kt):
                            r = kt - 4 * c
                            near = r >= -1
                            nm = nm_off[:, kt, :] if kt < 4 * c else nm_dg[:, r, :]
                            nmk = 'nm_off' if kt < 4 * c else 'nm_dg'
                            S_.op('pe', lambda e: e.matmul(sT[:], cT[:, kt * 128:(kt + 1) * 128], qlatT[:, h, :], start=True, stop=False),
                                  reads=['cT', 'qlatT'], writes=[sk])
                            S_.op('pe', lambda e: e.matmul(sT[:], self.ident[:], nm, start=False, stop=(not near)),
                                  reads=['ident', nmk], writes=[sk])
                            if near:
                                c0, c1 = max(r, 0) * 128, min(r + 2, 4) * 128
                                tb0 = 0 if r >= 0 else 128
                                S_.op('pe', lambda e: e.matmul(sT[:, c0:c1], self.ident[:], TB[:, h, tb0:tb0 + (c1 - c0)],
                                                               start=False, stop=True), reads=['ident', 'TB'], writes=[sk])
                            S_.op('act', lambda e: e.activation(out=pT[pj][:], in_=sT[:], func=AF.Exp, bias=cfar[:, h:h + 1], scale=sc),
                                  reads=[sk, 'cfar'], writes=['pT%d' % pj])

                        def pv(pj, h=h, kt=kt):
                            ap_, akey = ps[4 + h % 2], 'ps%d' % (4 + h % 2)
                            S_.op('pe', lambda e: e.matmul(ap_[:], vA[:, kt, h, :], pT[pj][:], start=(kt == 0), stop=(kt == nkt - 1)),
                                  reads=['vA', 'pT%d' % pj], writes=[akey])

                        def post(h=h, j=j, rows=rows):
                            ap_, akey = ps[4 + h % 2], 'ps%d' % (4 + h % 2)
                            rk = 'rd%d' % (h % 2)
                            S_.op('dve', lambda e: e.reciprocal(out=rd[rows, :], in_=ap_[64:128, :]), reads=[akey], writes=[rk])
                            S_.op('dve', lambda e: e.tensor_tensor(out=aoT[rows, j, :], in0=rd[rows, :], in1=ap_[0:64, :], op=ALU.mult),
                                  reads=[rk, akey], writes=['aoT%d' % (h % 2)])
                        blocks.append({'qk': qk, 'pv': pv, 'post': post if kt == nkt - 1 else None})
                for hb in range(8):
                    g, j = hb // 4, hb % 4
                    krows = slice(g * 64, (g + 1) * 64)
                    orows = slice((hb % 2) * 64, (hb % 2 + 1) * 64)
                    kts = [kt for kt in range(4 * c - 1, 4 * c + 4) if kt >= 0]
                    for kt in kts:
                        r = kt - 4 * c
                        c0, c1 = max(r, 0) * 128, min(r + 2, 4) * 128
                        tb0 = 0 if r >= 0 else 128

                        def qk(sT, sk, pj, hb=hb, kt=kt, c0=c0, c1=c1, tb0=tb0, krows=krows, j=j):
                            S_.op('pe', lambda e: e.matmul(sT[:, c0:c1], kbT[krows, kt * 128:(kt + 1) * 128], qbT[krows, j, c0:c1],
                                                           start=True, stop=False), reads=['kbT', 'qbT'], writes=[sk])
                            S_.op('pe', lambda e: e.matmul(sT[:, c0:c1], self.ident[:], TB[:, 8 + hb, tb0:tb0 + (c1 - c0)],
                                                           start=False, stop=True), reads=['ident', 'TB'], writes=[sk])
                            S_.op('act', lambda e: e.activation(out=pT[pj][:, c0:c1], in_=sT[:, c0:c1], func=AF.Exp, scale=sc),
                                  reads=[sk], writes=['pT%d' % pj])

                        def pv(pj, hb=hb, kt=kt, c0=c0, c1=c1, g=g):
                            ap_, akey = ps[4 + hb % 2], 'ps%d' % (4 + hb % 2)
                            for qq in range(c0 // 128, c1 // 128):
                                tqq = 4 * c + qq
                                first = (kt == max(tqq - 1, 0))
                                last = (kt == tqq)
                                qs = slice(qq * 128, (qq + 1) * 128)
                                S_.op('pe', lambda e: e.matmul(ap_[:, qs], vb[:, kt, g, :], pT[pj][:, qs],
                                                               start=first, stop=last), reads=['vb', 'pT%d' % pj], writes=[akey])

                        def post(hb=hb, orows=orows):
                            m2 = hb // 2
                            ap_, akey = ps[4 + hb % 2], 'ps%d' % (4 + hb % 2)
                            rk = 'rd%d' % (hb % 2)
                            S_.op('dve', lambda e: e.tensor_scalar(out=rd[orows, :], in0=ap_[64:128, :], scalar1=esink2[orows, m2:m2 + 1], scalar2=None,
                                                                   op0=ALU.add), reads=[akey, 'esink2'], writes=[rk])
                            S_.op('dve', lambda e: e.reciprocal(out=rd[orows, :], in_=rd[orows, :]), reads=[rk], writes=[rk])
                            S_.op('dve', lambda e: e.tensor_tensor(out=aoT[orows, 4 + m2, :], in0=rd[orows, :], in1=ap_[0:64, :], op=ALU.mult),
                                  reads=[rk, akey], writes=['aoT%d' % (hb % 2)])
                        blocks.append({'qk': qk, 'pv': pv, 'post': post if kt == kts[-1] else None})
                self.run_blocks(blocks)
                if STOP == 6:
                    S_.barrier()
                    return
                for tt in range(4):
                    for hf in range(2):
                        for j in range(KC):
                            S_.op('pe', lambda e: e.matmul(ps[hf][:], aoT[:, j, tt * 128:(tt + 1) * 128],
                                                           wout[:, j, hf * 512:(hf + 1) * 512], start=(j == 0), stop=(j == KC - 1)),
                                  reads=['aoT0', 'aoT1', 'wout'], writes=['ps%d' % hf])
                    self.ln_tile([ps[0][:], ps[1][:]], ['ps0', 'ps1'], 4 * c + tt, res_loc, out_loc)
            S_.barrier()

    def phase_mix1(self, b, res_loc, out_loc):
        S_, ps, nc = self.S, self.ps, self.nc
        W = self.t['od_w_in']
        r3 = lambda a: a.rearrange("(k p) n -> p k n", p=128)
        sc = 0.125
        with ExitStack() as st:
            wq = self.sb(st, 'wq', [128, KC, D], BF16)
            wgl = self.sb(st, 'wgl', [128, KC, 48], BF16)
            wout = self.sb(st, 'wout', [128, KC, D], BF16)
            kcmpT = self.sb(st, 'kcmpT', [128, 2, 128], BF16)
            vcmp = self.sb(st, 'vcmp', [128, 2, 128], BF16)
            ksE = [self.sb(st, 'ksE%d' % g, [128, S], BF16) for g in range(2)]
            kwT = self.sb(st, 'kwT', [128, 2, S], BF16)
            vs = self.sb(st, 'vs', [128, NT, 2, 128], BF16)
            vw = self.sb(st, 'vw', [128, NT, 2, 128], BF16)
            TB = self.sb(st, 'TB', [128, 16, 256], BF16)
            M4 = self.sb(st, 'M4', [128, 128], BF16)
            cfar = self.sb(st, 'cfar', [128, 16], F32)
            OV = self.sb(st, 'OV', [128, 33], BF16)
            gsel = self.sb(st, 'gsel', [48, 24, 128], BF16)
            zeros = self.sb(st, 'zeros', [128, 128], BF16)
            for hf in range(2):
                for j in range(8):
                    c0 = (hf * 8 + j) * 64
                    self.load_w(wq[:, :, j * 128 + hf * 64:j * 128 + (hf + 1) * 64], r3(W[:, c0:c0 + 64]), 'wq')
            self.load_w(wgl[:], r3(W[:, 1792:1840]), 'wgl')
            self.load_w(wout[:], r3(self.t['od_w_out']), 'wout')
            self.load_w(OV[0:127, :], self.t['ovl'], 'OV')
            self.load_w(gsel[:], self.t['gsel'], 'gsel')
            S_.dma('sp', cfar[:], self.bcast_rows(self.t['cfar'][0:1, :], 16), writes=['cfar'])
            S_.op('pool', lambda e: e.memset(zeros[:], 0.0), writes=['zeros'])
            S_.op('pool', lambda e: e.memset(kwT[:], 0.0), writes=['kwT'])
            S_.op('pool', lambda e: e.memset(kcmpT[:], 0.0), writes=['kcmpT'])
            for g in range(2):
                S_.op('pool', lambda e: e.memset(ksE[g][96:128, :], 0.0), writes=['ksE%d' % g])
            S_.op('pool', lambda e: e.memset(vs[:], 1.0), writes=['vs'])
            S_.op('pool', lambda e: e.memset(vw[:], 1.0), writes=['vw'])
            S_.op('pool', lambda e: e.memset(vcmp[:], 1.0), writes=['vcmp'])
            for g in range(2):
                self.load_w(ksE[g][64:96, :], self.t['eblk'], 'ksE%d' % g)
            S_.op('pool', lambda e: e.memset(M4[:], 0.0), writes=['M4'])
            S_.op('pool', lambda e: e.affine_select(out=M4[:], in_=M4[:], pattern=[[-1, 128]], compare_op=ALU.is_ge,
                                                      fill=NEG, base=-1, channel_multiplier=1), reads=['M4'], writes=['M4'])
            with ExitStack() as st2:
                bt = [self.sb(st2, 'bt%d' % i, [128, 256], F32) for i in range(2)]
                cm = self.sb(st2, 'cm', [128, 256], F32)
                S_.op('pool', lambda e: e.memset(cm[:], 0.0), writes=['cm'])
                S_.op('pool', lambda e: e.affine_select(out=cm[:, 0:128], in_=cm[:, 0:128], pattern=[[1, 128]], compare_op=ALU.is_ge,
                                                          fill=NEG, base=0, channel_multiplier=-1), reads=['cm'], writes=['cm'])
                for h in range(16):
                    k = 'bt%d' % (h % 2)
                    S_.dma('sp', bt[h % 2][:], self.t['bt_tab'][h], writes=[k])
                    S_.op('dve', lambda e: e.tensor_scalar(out=bt[h % 2][:], in0=bt[h % 2][:], scalar1=cfar[:, h:h + 1],
                                                           scalar2=8.0, op0=ALU.subtract, op1=ALU.mult),
                          reads=[k, 'cfar'], writes=[k])
                    S_.op('dve', lambda e: e.tensor_tensor(out=TB[:, h, :], in0=bt[h % 2][:], in1=cm[:], op=ALU.add),
                          reads=[k, 'cm'], writes=['TB'])
                S_.barrier()
            with ExitStack() as st2:
                wkf = self.sb(st2, 'wkf', [128, KC, 512], BF16)
                wtm = self.sb(st2, 'wtm', [128, KC, 256], BF16)
                w1 = [self.sb(st2, 'w1_%d' % i, [128, 32, 128], BF16) for i in range(2)]
                w2 = [self.sb(st2, 'w2_%d' % i, [128, 64], BF16) for i in range(2)]
                pe_sb = [self.sb(st2, 'pe%d' % i, [32, 64], BF16) for i in range(2)]
                peT = [self.sb(st2, 'peT%d' % i, [64, 32], BF16) for i in range(2)]
                crT = [self.sb(st2, 'crT%d' % i, [128, S], BF16) for i in range(2)]
                cpe = self.sb(st2, 'cpe', [128, 2], F32)
                u = self.sb(st2, 'u', [128, 128], F32)
                u2 = self.sb(st2, 'u2', [128, 128], F32)
                th = self.sb(st2, 'th', [128, 128], F32)
                hb_ = self.sb(st2, 'hidb', [128, 128], BF16)
                self.load_w(wkf[:, :, 0:256], r3(W[:, 1024:1280]), 'wkf')
                self.load_w(wkf[:, :, 256:384], r3(W[:, 1280:1408]), 'wkf')
                self.load_w(wkf[:, :, 384:512], r3(W[:, 1536:1664]), 'wkf')
                self.load_w(wtm[:, :, 0:128], r3(W[:, 1408:1536]), 'wtm')
                self.load_w(wtm[:, :, 128:256], r3(W[:, 1664:1792]), 'wtm')
                for i, nm in enumerate(('k', 'v')):
                    for g in range(2):
                        self.load_w(w1[i][g * 64:(g + 1) * 64, :, :],
                                    self.t['od_w1_' + nm].rearrange("(l d) j -> d l j", d=64), 'w1_%d' % i)
                    self.load_w(w2[i][:], self.t['od_w2_' + nm], 'w2_%d' % i)
                    self.load_w(pe_sb[i][:], self.t['od_pe_' + nm], 'pe%d' % i)
                    S_.op('pe', lambda e: e.transpose(self.pt[0:64, 0:32], pe_sb[i][:], self.ident[0:32, 0:32]),
                          reads=['pe%d' % i, 'ident'], writes=['pt'])
                    S_.op('dve', lambda e: e.tensor_copy(out=peT[i][:], in_=self.pt[0:64, 0:32]), reads=['pt'], writes=['peT%d' % i])
                    for l in range(32):
                        S_.op('pe', lambda e: e.matmul(ps[6][:, i:i + 1], w1[i][0:64, l, :], peT[i][:, l:l + 1],
                                                       start=(l == 0), stop=(l == 31)), reads=['w1_%d' % i, 'peT%d' % i], writes=['ps6'])
                    S_.op('dve', lambda e: e.tensor_copy(out=cpe[:, i:i + 1], in_=ps[6][:, i:i + 1]), reads=['ps6'], writes=['cpe'])
                for t in range(NT):
                    hk = 'hT%d' % (t // 4)
                    for kc in range(KC):
                        S_.op('pe', lambda e: e.matmul(ps[0][:, 0:256], self.hT[:, kc, t * 128:(t + 1) * 128], wtm[:, kc, :],
                                                       start=(kc == 0), stop=(kc == KC - 1)), reads=[hk, 'wtm'], writes=['ps0'])
                    S_.op('act', lambda e: e.activation(out=vs[:, t, :, 0:64], in_=ps[0][:, 0:128].rearrange("p (g d) -> p g d", g=2),
                                                        func=AF.Copy), reads=['ps0'], writes=['vs'])
                    S_.op('dve', lambda e: e.tensor_copy(out=vw[:, t, :, 0:64], in_=ps[0][:, 128:256].rearrange("p (g d) -> p g d", g=2)),
                          reads=['ps0'], writes=['vw'])
                dsts = [(crT[0], 'crT0'), (crT[1], 'crT1'), (None, 'ks'), (None, 'kw')]
                n_ = 0
                for cc in range(4):
                    hk = 'hT%d' % cc
                    sl = slice(cc * 512, (cc + 1) * 512)
                    for gi, (dst, dk) in enumerate(dsts):
                        pj = 1 + (n_ % 2)
                        n_ += 1
                        for kc in range(KC):
                            S_.op('pe', lambda e: e.matmul(ps[pj][:], wkf[:, kc, gi * 128:(gi + 1) * 128], self.hT[:, kc, sl],
                                                           start=(kc == 0), stop=(kc == KC - 1)), reads=[hk, 'wkf'], writes=['ps%d' % pj])
                        if dk == 'ks':
                            S_.op('act', lambda e: e.activation(out=ksE[0][0:64, sl], in_=ps[pj][0:64, :], func=AF.Copy), reads=['ps%d' % pj], writes=['ksE0'])
                            S_.op('act', lambda e: e.activation(out=ksE[1][0:64, sl], in_=ps[pj][64:128, :], func=AF.Copy), reads=['ps%d' % pj], writes=['ksE1'])
                        elif dk == 'kw':
                            S_.op('dve', lambda e: e.tensor_copy(out=kwT[0:64, 0, sl], in_=ps[pj][0:64, :]), reads=['ps%d' % pj], writes=['kwT'])
                            S_.op('dve', lambda e: e.tensor_copy(out=kwT[64:128, 1, sl], in_=ps[pj][64:128, :]), reads=['ps%d' % pj], writes=['kwT'])
                        elif gi % 2 == 0:
                            S_.op('act', lambda e: e.activation(out=dst[:, sl], in_=ps[pj][:], func=AF.Copy), reads=['ps%d' % pj], writes=[dk])
                        else:
                            S_.op('dve', lambda e: e.tensor_copy(out=dst[:, sl], in_=ps[pj][:]), reads=['ps%d' % pj], writes=[dk])
                for i in range(2):
                    for g in range(2):
                        rows = slice(g * 64, (g + 1) * 64)
                        for l in range(32):
                            S_.op('pe', lambda e: e.matmul(ps[3][:, 0:127], w1[i][rows, l, :], crT[i][rows, l:l + 16 * 126 + 1:16],
                                                           start=(l == 0), stop=(l == 31)), reads=['w1_%d' % i, 'crT%d' % i], writes=['ps3'])
                        S_.op('act', lambda e: e.activation(out=u[:, 0:127], in_=ps[3][:, 0:127], func=AF.Identity, bias=cpe[:, i:i + 1]),
                              reads=['ps3', 'cpe'], writes=['u'])
                        S_.op('dve', lambda e: e.tensor_tensor(out=u2[:, 0:127], in0=u[:, 0:127], in1=u[:, 0:127], op=ALU.mult),
                              reads=['u'], writes=['u2'])
                        S_.op('dve', lambda e: e.tensor_scalar(out=u2[:, 0:127], in0=u2[:, 0:127], scalar1=0.044715, scalar2=1.0,
                                                               op0=ALU.mult, op1=ALU.add), reads=['u2'], writes=['u2'])
                        S_.op('dve', lambda e: e.tensor_tensor(out=u2[:, 0:127], in0=u2[:, 0:127], in1=u[:, 0:127], op=ALU.mult),
                              reads=['u', 'u2'], writes=['u2'])
                        S_.op('act', lambda e: e.activation(out=th[:, 0:127], in_=u2[:, 0:127], func=AF.Tanh, scale=0.7978845608028654),
                              reads=['u2'], writes=['th'])
                        S_.op('dve', lambda e: e.scalar_tensor_tensor(out=th[:, 0:127], in0=th[:, 0:127], scalar=1.0, in1=u[:, 0:127],
                                                                      op0=ALU.add, op1=ALU.mult), reads=['th', 'u'], writes=['th'])
                        S_.op('dve', lambda e: e.tensor_scalar(out=hb_[:, 0:127], in0=th[:, 0:127], scalar1=0.5, scalar2=None, op0=ALU.mult),
                              reads=['th'], writes=['hidb'])
                        if i == 0:
                            S_.op('pe', lambda e: e.matmul(ps[4][rows, 0:127], w2[0][:], hb_[:, 0:127], start=True, stop=True),
                                  reads=['w2_0', 'hidb'], writes=['ps4'])
                            S_.op('act', lambda e: e.activation(out=kcmpT[rows, g, 0:127], in_=ps[4][rows, 0:127], func=AF.Copy),
                                  reads=['ps4'], writes=['kcmpT'])
                        else:
                            S_.op('pe', lambda e: e.matmul(ps[5][0:127, g * 64:(g + 1) * 64], hb_[:, 0:127], w2[1][:], start=True, stop=True),
                                  reads=['w2_1', 'hidb'], writes=['ps5'])
                            S_.op('act', lambda e: e.activation(out=vcmp[0:127, g, 0:64], in_=ps[5][0:127, g * 64:(g + 1) * 64], func=AF.Copy),
                                  reads=['ps5'], writes=['vcmp'])
                S_.barrier()
            qT = self.sb(st, 'qT', [128, 8, 512], BF16)
            qS = self.sb(st, 'qS', [128, 16, 512], BF16)
            S_.op('pool', lambda e: e.memset(qS[96:128, :, :], 0.0), writes=['qS'])
            gT = self.sb(st, 'gT', [48, 512], BF16)
            gsb = [self.sb(st, 'gsb%d' % i, [128, 512], F32) for i in range(2)]
            cbias = [self.sb(st, 'cbias%d' % i, [128, 512], F32) for i in range(2)]
            s_sb = [self.sb(st, 's_sb%d' % i, [128, 512], F32) for i in range(2)]
            pT = [self.sb(st, 'pT%d' % i, [128, 512], BF16) for i in range(2)]
            rd = self.sb(st, 'rd', [128, 512], F32)
            to = self.sb(st, 'to', [128, 512], F32)
            acc = self.sb(st, 'acc', [128, 8, 512], F32)
            aoT = self.sb(st, 'aoT', [128, 8, 512], BF16)
            pslc = self.sb(st, 'pslc', [128, 4, 64], F32)
            stab = self.sb(st, 'stab', [128, 3, 4, 64], F32)
            sscore = self.sb(st, 'sscore', [128, 64], F32)
            top = self.sb(st, 'top', [128, 16], F32)
            rq = self.sb(st, 'rq', [128, 8], F32)
            nsel = self.sb(st, 'nsel', [128, 64], BF16)
            self.ln_setup(st, 1, 0)
            pcnt = 0
            for c in range(4):
                hk = 'hT%d' % c
                qsl = slice(c * 512, (c + 1) * 512)
                for k3 in range(3):
                    S_.dma('sp', stab[:, k3, :, :], self.t['seltab'][k3, 4 * c:4 * c + 4].rearrange("t p n -> p t n"), writes=['stab'])
                for j in range(8):
                    pj = j % 2
                    for kc in range(KC):
                        S_.op('pe', lambda e: e.matmul(ps[pj][:], wq[:, kc, j * 128:(j + 1) * 128], self.hT[:, kc, qsl],
                                                       start=(kc == 0), stop=(kc == KC - 1)), reads=[hk, 'wq'], writes=['ps%d' % pj])
                    if j % 2 == 0:
                        S_.op('act', lambda e: e.activation(out=qT[:, j, :], in_=ps[pj][:], func=AF.Copy), reads=['ps%d' % pj], writes=['qT'])
                    else:
                        S_.op('dve', lambda e: e.tensor_copy(out=qT[:, j, :], in_=ps[pj][:]), reads=['ps%d' % pj], writes=['qT'])
                    S_.op('dve', lambda e: e.tensor_copy(out=qS[0:64, j, :], in_=ps[pj][0:64, :]), reads=['ps%d' % pj], writes=['qS'])
                    S_.op('act', lambda e: e.activation(out=qS[0:64, 8 + j, :], in_=ps[pj][64:128, :], func=AF.Copy), reads=['ps%d' % pj], writes=['qS'])
                for kc in range(KC):
                    S_.op('pe', lambda e: e.matmul(ps[0][0:48, :], wgl[:, kc, :], self.hT[:, kc, qsl],
                                                   start=(kc == 0), stop=(kc == KC - 1)), reads=[hk, 'wgl'], writes=['ps0'])
                S_.op('act', lambda e: e.activation(out=gT[:], in_=ps[0][0:48, :], func=AF.Copy), reads=['ps0'], writes=['gT'])

                def gate_pair(m, br):
                    gi = (m * 3 + br) % 2
                    S_.op('pe', lambda e: e.matmul(ps[0][:], gsel[:, m * 3 + br, :], gT[:], start=True, stop=True),
                          reads=['gsel', 'gT'], writes=['ps0'])
                    S_.op('act', lambda e: e.activation(out=gsb[gi][:], in_=ps[0][:], func=AF.Exp, scale=-1.0), reads=['ps0'], writes=['gsb%d' % gi])
                    return gi

                def finish_head(h, br, gi, acc_ps, acc_key, tiny):
                    m = h // 2
                    R = slice((h % 2) * 64, (h % 2 + 1) * 64)
                    ak = 'acc%d_%d' % (m, h % 2)
                    if tiny:
                        S_.op('dve', lambda e: e.tensor_scalar(out=rd[R, :], in0=acc_ps[64:128, :], scalar1=1e-30, scalar2=None, op0=ALU.add),
                              reads=[acc_key], writes=['rd%d' % (h % 2)])
                        S_.op('dve', lambda e: e.scalar_tensor_tensor(out=rd[R, :], in0=gsb[gi][R, :], scalar=1.0, in1=rd[R, :],
                                                                      op0=ALU.add, op1=ALU.mult),
                              reads=['gsb%d' % gi, 'rd%d' % (h % 2)], writes=['rd%d' % (h % 2)])
                    else:
                        S_.op('dve', lambda e: e.scalar_tensor_tensor(out=rd[R, :], in0=gsb[gi][R, :], scalar=1.0, in1=acc_ps[64:128, :],
                                                                      op0=ALU.add, op1=ALU.mult),
                              reads=['gsb%d' % gi, acc_key], writes=['rd%d' % (h % 2)])
                    S_.op('dve', lambda e: e.reciprocal(out=rd[R, :], in_=rd[R, :]), reads=['rd%d' % (h % 2)], writes=['rd%d' % (h % 2)])
                    if br == 0:
                        S_.op('dve', lambda e: e.tensor_tensor(out=acc[R, m, :], in0=rd[R, :], in1=acc_ps[0:64, :], op=ALU.mult),
                              reads=['rd%d' % (h % 2), acc_key], writes=[ak])
                    else:
                        S_.op('dve', lambda e: e.tensor_tensor(out=to[R, :], in0=rd[R, :], in1=acc_ps[0:64, :], op=ALU.mult),
                              reads=['rd%d' % (h % 2), acc_key], writes=['to%d' % (h % 2)])
                        S_.op('pool', lambda e: e.tensor_tensor(out=acc[R, m, :], in0=acc[R, m, :], in1=to[R, :], op=ALU.add),
                              reads=['to%d' % (h % 2), ak], writes=[ak])

                def head_info(h):
                    g = h // 8
                    return g, h % 8, slice(g * 64, (g + 1) * 64), slice((h % 2) * 64, (h % 2 + 1) * 64)

                blocks = []
                gstate = {}
                for m in range(8):
                    for hh in range(2):
                        h = 2 * m + hh
                        g, j, qrows, orows = head_info(h)

                        def qk(sT, sk, pj, h=h, g=g, j=j, qrows=qrows):
                            S_.dma('sp', cbias[pj][0:127, :], self.t['cb_tab'][h, c], writes=['cbias%d' % pj])
                            S_.op('pe', lambda e: e.matmul(sT[0:127, :], kcmpT[:, g, 0:127], qT[:, j, :], start=True, stop=True),
                                  reads=['kcmpT', 'qT'], writes=[sk])
                            S_.op('dve', lambda e: e.scalar_tensor_tensor(out=s_sb[pj][0:127, :], in0=sT[0:127, :], scalar=sc,
                                                                          in1=cbias[pj][0:127, :], op0=ALU.mult, op1=ALU.add),
                                  reads=[sk, 'cbias%d' % pj], writes=['s_sb%d' % pj])
                            S_.op('act', lambda e: e.activation(out=pT[pj][0:127, :], in_=s_sb[pj][0:127, :], func=AF.Exp),
                                  reads=['s_sb%d' % pj], writes=['pT%d' % pj])

                        def pv(pj, h=h, g=g, orows=orows):
                            ap_, akey = ps[4 + h % 2], 'ps%d' % (4 + h % 2)
                            S_.op('pe', lambda e: e.matmul(ap_[:], vcmp[0:127, g, :], pT[pj][0:127, :], start=True, stop=True),
                                  reads=['vcmp', 'pT%d' % pj], writes=[akey])
                            for tt in range(4):
                                S_.op('pe', lambda e: e.matmul(ps[1][:, tt * 64:tt * 64 + 33], pT[pj][0:127, tt * 128:(tt + 1) * 128], OV[0:127, :],
                                                               start=True, stop=True), reads=['OV', 'pT%d' % pj], writes=['ps1'])
                            S_.op('dve', lambda e: e.tensor_scalar(out=rq[:, 0:4], in0=ps[1][:].rearrange("p (t n) -> p t n", n=64)[:, 0:4, 32],
                                                                   scalar1=1e-30, scalar2=None, op0=ALU.add), reads=['ps1'], writes=['rq'])
                            S_.op('dve', lambda e: e.reciprocal(out=rq[:, 0:4], in_=rq[:, 0:4]), reads=['rq'], writes=['rq'])
                            for tt in range(4):
                                dst = pslc[:, tt, g * 32:(g + 1) * 32]
                                if h % 8 == 0:
                                    S_.op('dve', lambda e: e.tensor_scalar(out=dst, in0=ps[1][:, tt * 64:tt * 64 + 32], scalar1=rq[:, tt:tt + 1],
                                                                           scalar2=None, op0=ALU.mult), reads=['ps1', 'rq'], writes=['pslc'])
                                else:
                                    S_.op('dve', lambda e: e.scalar_tensor_tensor(out=dst, in0=ps[1][:, tt * 64:tt * 64 + 32], scalar=rq[:, tt:tt + 1],
                                                                                  in1=dst, op0=ALU.mult, op1=ALU.add),
                                          reads=['ps1', 'rq', 'pslc'], writes=['pslc'])

                        def post(h=h, m=m, hh=hh):
                            if hh == 0:
                                gstate['gi'] = gate_pair(m, 0)
                            finish_head(h, 0, gstate['gi'], ps[4 + h % 2], 'ps%d' % (4 + h % 2), True)
                        blocks.append({'qk': qk, 'pv': pv, 'post': post})
                self.run_blocks(blocks)
                for tt in range(4):
                    S_.op('dve', lambda e: e.tensor_tensor(out=sscore[:], in0=pslc[:, tt, :], in1=stab[:, 0, tt, :], op=ALU.add),
                          reads=['pslc', 'stab'], writes=['sscore'])
                    S_.op('dve', lambda e: e.tensor_tensor(out=sscore[:], in0=sscore[:], in1=stab[:, 1, tt, :], op=ALU.mult),
                          reads=['sscore', 'stab'], writes=['sscore'])
                    S_.op('dve', lambda e: e.tensor_tensor(out=sscore[:], in0=sscore[:], in1=stab[:, 2, tt, :], op=ALU.add),
                          reads=['sscore', 'stab'], writes=['sscore'])
                    for g in range(2):
                        S_.op('dve', lambda e: e.max(out=top[:, g * 8:(g + 1) * 8], in_=sscore[:, g * 32:(g + 1) * 32]),
                              reads=['sscore'], writes=['top'])
                    for g in range(2):
                        S_.op('dve', lambda e: e.tensor_scalar(out=nsel[:, g * 32:(g + 1) * 32], in0=sscore[:, g * 32:(g + 1) * 32],
                                                               scalar1=top[:, g * 8 + 7:g * 8 + 8], scalar2=-1024.0, op0=ALU.is_lt, op1=ALU.mult),
                              reads=['sscore', 'top'], writes=['nsel'])
                    for g in range(2):
                        S_.op('pe', lambda e: e.transpose(self.pt[64:96, g * 128:(g + 1) * 128], nsel[:, g * 32:(g + 1) * 32], self.ident[:]),
                              reads=['nsel', 'ident'], writes=['pt'])
                    for g in range(2):
                        src0 = self.pt[64:96, g * 128:(g + 1) * 128]
                        src = bass.AP(src0.tensor, src0.offset, [list(src0.ap[0]), [0, 8], [1, 128]])
                        if g == 0:
                            S_.op('act', lambda e: e.activation(out=qS[64:96, 0:8, tt * 128:(tt + 1) * 128], in_=src, func=AF.Copy),
                                  reads=['pt'], writes=['qS'])
                        else:
                            S_.op('dve', lambda e: e.tensor_copy(out=qS[64:96, 8:16, tt * 128:(tt + 1) * 128], in_=src),
                                  reads=['pt'], writes=['qS'])
                blocks = []
                for br in (1, 2):
                    for m in range(8):
                        for hh in range(2):
                            h = 2 * m + hh
                            g, j, qrows, orows = head_info(h)
                            kts = list(range(0, 4 * c + 4)) if br == 1 else list(range(max(4 * c - 4, 0), 4 * c + 4))
                            for ki, kt in enumerate(kts):
                                r = kt - 4 * c
                                lo = max(r, 0) * 128
                                hi = 512 if br == 1 else (min(r + 4, 3) + 1) * 128

                                def qk(sT, sk, pj, br=br, h=h, g=g, j=j, qrows=qrows, kt=kt, r=r, lo=lo, hi=hi):
                                    near = r >= -1
                                    m4 = (br == 2 and r <= -1)
                                    if br == 1:
                                        S_.op('pe', lambda e: e.matmul(sT[:, lo:hi], ksE[g][:, kt * 128:(kt + 1) * 128], qS[:, h, lo:hi],
                                                                       start=True, stop=(not near)), reads=['ksE%d' % g, 'qS'], writes=[sk])
                                    else:
                                        S_.op('pe', lambda e: e.matmul(sT[:, lo:hi], kwT[:, g, kt * 128:(kt + 1) * 128], qT[:, j, lo:hi],
                                                                       start=True, stop=(not near and not m4)), reads=['kwT', 'qT'], writes=[sk])
                                    if near:
                                        c0, c1 = max(r, 0) * 128, min(r + 2, 4) * 128
                                        tb0 = 0 if r >= 0 else 128
                                        S_.op('pe', lambda e: e.matmul(sT[:, c0:c1], self.ident[:], TB[:, h, tb0:tb0 + (c1 - c0)],
                                                                       start=False, stop=(not m4)), reads=['ident', 'TB'], writes=[sk])
                                    if m4:
                                        q4 = (r + 4) * 128
                                        S_.op('pe', lambda e: e.matmul(sT[:, q4:q4 + 128], self.ident[:], M4[:], start=False, stop=True),
                                              reads=['ident', 'M4'], writes=[sk])
                                    S_.op('act', lambda e: e.activation(out=pT[pj][:, lo:hi], in_=sT[:, lo:hi], func=AF.Exp, bias=cfar[:, h:h + 1], scale=sc),
                                          reads=[sk, 'cfar'], writes=['pT%d' % pj])

                                def pv(pj, br=br, h=h, g=g, kt=kt, lo=lo, hi=hi, ki=ki, nk_=len(kts)):
                                    vv = vs if br == 1 else vw
                                    vvn = 'vs' if br == 1 else 'vw'
                                    ap_, akey = ps[4 + h % 2], 'ps%d' % (4 + h % 2)
                                    full0 = (ki == 0 and lo == 0 and hi == 512)
                                    if ki == 0 and not full0:
                                        S_.op('pe', lambda e: e.matmul(ap_[:], zeros[:], qT[:, 0, :], start=True, stop=False),
                                              reads=['zeros', 'qT'], writes=[akey])
                                    lastk = (ki == nk_ - 1)
                                    S_.op('pe', lambda e: e.matmul(ap_[:, lo:hi], vv[:, kt, g, :], pT[pj][:, lo:hi],
                                                                   start=full0, stop=lastk), reads=[vvn, 'pT%d' % pj], writes=[akey])

                                def post(h=h, m=m, hh=hh, br=br):
                                    if hh == 0:
                                        gstate['gi'] = gate_pair(m, br)
                                    finish_head(h, br, gstate['gi'], ps[4 + h % 2], 'ps%d' % (4 + h % 2), False)
                                blocks.append({'qk': qk, 'pv': pv, 'post': post if ki == len(kts) - 1 else None})
                self.run_blocks(blocks)
                for m in range(8):
                    if m % 2 == 0:
                        S_.op('act', lambda e: e.activation(out=aoT[:, m, :], in_=acc[:, m, :], func=AF.Copy), reads=['acc%d_0' % m, 'acc%d_1' % m], writes=['aoT'])
                    else:
                        S_.op('dve', lambda e: e.tensor_copy(out=aoT[:, m, :], in_=acc[:, m, :]), reads=['acc%d_0' % m, 'acc%d_1' % m], writes=['aoT'])
                for tt in range(4):
                    for hf in range(2):
                        for j in range(KC):
                            S_.op('pe', lambda e: e.matmul(ps[hf][:], aoT[:, j, tt * 128:(tt + 1) * 128],
                                                           wout[:, j, hf * 512:(hf + 1) * 512], start=(j == 0), stop=(j == KC - 1)),
                                  reads=['aoT', 'wout'], writes=['ps%d' % hf])
                    self.ln_tile([ps[0][:], ps[1][:]], ['ps0', 'ps1'], 4 * c + tt, res_loc, out_loc)
            S_.barrier()

    def phase_ffn(self, b, res_loc, out_loc, moe):
        S_, ps = self.S, self.ps
        G = 4 if moe else 2
        nexp = N_EXP if moe else 1
        ngrp = (D_FFE if moe else D_FF) // (128 * G)
        with ExitStack() as st:
            yacc = self.sb(st, 'yacc', [128, NT, D], F32)
            wg = [self.sb(st, 'wg%d' % j, [128, KC, G * 128], BF16) for j in range(2)]
            wu = [self.sb(st, 'wu%d' % j, [128, KC, G * 128], BF16) for j in range(2)]
            wd = [self.sb(st, 'wd%d' % j, [128, G, D], BF16) for j in range(2)]
            if moe:
                wr = self.sb(st, 'wr', [128, KC, N_EXP], BF16)
                gate = self.sb(st, 'gate', [128, NT, N_EXP], F32)
                lg = self.sb(st, 'rlg', [128, 8], F32)
                top = self.sb(st, 'rtop', [128, 8], F32)
                wk_ = self.sb(st, 'rwk', [128, 8], F32)
                m1 = self.sb(st, 'rm1', [128, 8], F32)
                m2 = self.sb(st, 'rm2', [128, 8], F32)
                self.load_w(wr[:], self.t['moe_w_router'].rearrange("(k p) n -> p k n", p=128), 'wr')
                def emit_router(t):
                    for kc in range(KC):
                        S_.op('pe', lambda e: e.matmul(ps[6][:, 0:8], self.hT[:, kc, t * 128:(t + 1) * 128], wr[:, kc, :],
                                                       start=(kc == 0), stop=(kc == KC - 1)),
                              reads=['wr', 'hT%d' % (t // 4)], writes=['ps6'])
                    S_.op('dve', lambda e: e.tensor_copy(out=lg[:], in_=ps[6][:, 0:8]), reads=['ps6'], writes=['rlg'])
                    S_.op('dve', lambda e: e.max(out=top[:], in_=lg[:]), reads=['rlg'], writes=['rtop'])
                    S_.op('dve', lambda e: e.tensor_tensor(out=wk_[:, 0:1], in0=top[:, 0:1], in1=top[:, 1:2], op=ALU.subtract),
                          reads=['rtop'], writes=['rwk'])
                    S_.op('act', lambda e: e.activation(out=wk_[:, 1:2], in_=wk_[:, 0:1], func=AF.Sigmoid),
                          reads=['rwk'], writes=['rwk'])
                    S_.op('dve', lambda e: e.tensor_scalar(out=wk_[:, 2:3], in0=wk_[:, 1:2], scalar1=-1.0, scalar2=1.0,
                                                           op0=ALU.mult, op1=ALU.add), reads=['rwk'], writes=['rwk'])
                    S_.op('dve', lambda e: e.tensor_scalar(out=m1[:], in0=lg[:], scalar1=top[:, 0:1], scalar2=wk_[:, 1:2],
                                                           op0=ALU.is_equal, op1=ALU.mult), reads=['rlg', 'rtop', 'rwk'],
                          writes=['rm1'])
                    S_.op('dve', lambda e: e.tensor_scalar(out=m2[:], in0=lg[:], scalar1=top[:, 1:2], scalar2=wk_[:, 2:3],
                                                           op0=ALU.is_equal, op1=ALU.mult), reads=['rlg', 'rtop', 'rwk'],
                          writes=['rm2'])
                    S_.op('dve', lambda e: e.tensor_tensor(out=gate[:, t, :], in0=m1[:], in1=m2[:], op=ALU.add),
                          reads=['rm1', 'rm2'], writes=['gate'])

            def wsrc(ex, g):
                c0 = g * G * 128
                if moe:
                    return (self.t['moe_w_gate'][ex, :, c0:c0 + G * 128], self.t['moe_w_up'][ex, :, c0:c0 + G * 128],
                            self.t['moe_w_down'][ex, c0:c0 + G * 128, :])
                return (self.t['ff_w_gate'][:, c0:c0 + G * 128], self.t['ff_w_up'][:, c0:c0 + G * 128],
                        self.t['ff_w_down'][c0:c0 + G * 128, :])

            def load_group(idx, ex, g):
                j = idx % 2
                a, u_, d_ = wsrc(ex, g)
                self.load_w(wg[j][:], a.rearrange("(k p) n -> p k n", p=128), 'wg%d' % j)
                self.load_w(wu[j][:], u_.rearrange("(k p) n -> p k n", p=128), 'wu%d' % j)
                self.load_w(wd[j][:], d_.rearrange("(g p) n -> p g n", p=128), 'wd%d' % j)

            groups = [(ex, g) for ex in range(nexp) for g in range(ngrp)]
            sg5 = [self.sb(st, 'sg5_%d' % j_, [128, 512], F32) for j_ in range(2)]
            abuf = [self.sb(st, 'abuf%d' % j_, [128, G, 512], BF16) for j_ in range(2)]
            ptf = self.pt[:].bitcast(F32)
            ybanks = [ps[4][:], ps[5][:], ps[6][:], ptf]
            ykeys = ['ps4', 'ps5', 'ps6', 'pt']
            load_group(0, *groups[0])
            cnt = [0]
            for idx, (ex, g) in enumerate(groups):
                if idx + 1 < len(groups):
                    load_group(idx + 1, *groups[idx + 1])
                j = idx % 2

                def emit_gu(tb, ci):
                    pj = cnt[0] % 2
                    cnt[0] += 1
                    pg, pu = ps[2 * pj], ps[2 * pj + 1]
                    kg, ku = 'ps%d' % (2 * pj), 'ps%d' % (2 * pj + 1)
                    hk = 'hT%d' % tb
                    tok = slice(tb * 512, (tb + 1) * 512)
                    for kc in range(KC):
                        S_.op('pe', lambda e: e.matmul(pg[:], wg[j][:, kc, ci * 128:(ci + 1) * 128], self.hT[:, kc, tok],
                                                       start=(kc == 0), stop=(kc == KC - 1)), reads=['wg%d' % j, hk], writes=[kg])
                    for kc in range(KC):
                        S_.op('pe', lambda e: e.matmul(pu[:], wu[j][:, kc, ci * 128:(ci + 1) * 128], self.hT[:, kc, tok],
                                                       start=(kc == 0), stop=(kc == KC - 1)), reads=['wu%d' % j, hk], writes=[ku])
                    S_.op('act', lambda e: e.activation(out=sg5[pj][:], in_=pg[:], func=AF.Silu), reads=[kg], writes=['sg5_%d' % pj])
                    S_.op('dve', lambda e: e.tensor_tensor(out=abuf[tb % 2][:, ci, :], in0=sg5[pj][:], in1=pu[:], op=ALU.mult),
                          reads=['sg5_%d' % pj, ku], writes=['abuf%d' % (tb % 2)])

                def emit_down(tb, hp):
                    ab, abk = abuf[tb % 2], 'abuf%d' % (tb % 2)
                    for ci in range(G):
                        for tt in range(2):
                            col = (hp * 2 + tt) * 128
                            for hf in range(2):
                                bi = tt * 2 + hf
                                S_.op('pe', lambda e: e.matmul(ybanks[bi], ab[:, ci, col:col + 128], wd[j][:, ci, hf * 512:(hf + 1) * 512],
                                                               start=(ci == 0), stop=(ci == G - 1)), reads=[abk, 'wd%d' % j], writes=[ykeys[bi]])
                    for tt in range(2):
                        t = tb * 4 + hp * 2 + tt
                        for hf in range(2):
                            bi = tt * 2 + hf
                            src, sk = ybanks[bi], ykeys[bi]
                            dst = yacc[:, t, hf * 512:(hf + 1) * 512]
                            yk = 'yacc%d' % t
                            if moe:
                                if idx == 0:
                                    S_.op('dve', lambda e: e.tensor_scalar(out=dst, in0=src, scalar1=gate[:, t, ex:ex + 1],
                                                                           scalar2=None, op0=ALU.mult), reads=[sk, 'gate'], writes=[yk])
                                else:
                                    S_.op('dve', lambda e: e.scalar_tensor_tensor(out=dst, in0=src, scalar=gate[:, t, ex:ex + 1], in1=dst,
                                                                                  op0=ALU.mult, op1=ALU.add), reads=[sk, 'gate', yk], writes=[yk])
                            else:
                                if idx == 0:
                                    S_.op('dve', lambda e: e.tensor_copy(out=dst, in_=src), reads=[sk], writes=[yk])
                                else:
                                    S_.op('dve', lambda e: e.tensor_tensor(out=dst, in0=dst, in1=src, op=ALU.add), reads=[sk, yk], writes=[yk])

                pending = None
                for tb in range(4):
                    for ci in range(G):
                        emit_gu(tb, ci)
                        if moe and idx == 0:
                            emit_router(tb * G + ci)
                        if pending is not None and ci == 0:
                            emit_down(pending, 0)
                        if pending is not None and ci == G // 2:
                            emit_down(pending, 1)
                    pending = tb
                emit_down(3, 0)
                emit_down(3, 1)
            self.ln_setup(st, 1 if moe else 0, 2)
            for t in range(NT):
                self.ln_tile([yacc[:, t, 0:512], yacc[:, t, 512:1024]], ['yacc%d' % t] * 2, t, res_loc, out_loc,
                             want_hT=(out_loc[0] != 'out'))
            S_.barrier()


_PROG_CACHE = {}

FULL_PHASES = ['x0', 'mix0', 'xattn0', 'ffn', 'mix1', 'xattn1', 'moe']


def make_inputs(inputs, phases=None):
    f = lambda a: np.ascontiguousarray(np.asarray(a, dtype=np.float32))
    rel = f(inputs['rel_table'])
    kk = np.arange(128)[:, None]
    jj_ = np.arange(256)[None, :]
    bt_tab = np.ascontiguousarray(rel[_rel_bucket_np(jj_ - kk), :].transpose(2, 0, 1))
    cfar = np.ascontiguousarray(rel[31:32, :])
    u = (np.arange(S)[None, :] - 16 * np.arange(127)[:, None] - 31)
    cb_tab = np.where(u[None] < 0, np.float32(NEG), rel[_rel_bucket_np(u), :].transpose(2, 0, 1)).astype(np.float32)
    cb_tab = np.ascontiguousarray(cb_tab.reshape(16, 127, 4, 512).transpose(0, 2, 1, 3))
    n = np.arange(127)[:, None] * 16
    jb = np.arange(32)[None, :] * 64
    ovl = np.concatenate([((n < jb + 64) & (n + 32 > jb)).astype(np.float32), np.ones((127, 1), np.float32)], axis=1)
    eblk = (np.arange(S)[None, :] // 64 == np.arange(32)[:, None]).astype(np.float32)
    gsel = np.zeros((48, 24, 128), np.float32)
    for m in range(8):
        for br in range(3):
            gsel[(2 * m) * 3 + br, m * 3 + br, 0:64] = 1.0
            gsel[(2 * m + 1) * 3 + br, m * 3 + br, 64:128] = 1.0
    pos = np.arange(S)
    blk = pos // 64
    jj = np.arange(32)
    forced = (jj[None, :] == 0) | (jj[None, :] == blk[:, None]) | (jj[None, :] == blk[:, None] - 1)
    adm = jj[None, :] * 64 <= pos[:, None]
    seltab = np.zeros((3, S, 64), np.float32)
    for g in range(2):
        seltab[0, :, g * 32:(g + 1) * 32] = forced.astype(np.float32) * 1e6
        seltab[1, :, g * 32:(g + 1) * 32] = adm.astype(np.float32)
        seltab[2, :, g * 32:(g + 1) * 32] = np.where(adm, 0.0, -1e30)
    seltab = seltab.reshape(3, 16, 128, 64)
    shared = {
        'ev_w_in': f(inputs['ev_w_in'][0]), 'ev_ckv_gain': f(inputs['ev_ckv_gain']), 'ev_w_uk': f(inputs['ev_w_uk'][0]),
        'ev_w_uv': f(inputs['ev_w_uv'][0]), 'ev_sinks': f(inputs['ev_sinks']), 'ev_w_out': f(inputs['ev_w_out'][0]),
        'od_w_in': f(inputs['od_w_in'][0]), 'od_pe_k': f(inputs['od_pe_k'][0]), 'od_w1_k': f(inputs['od_w1_k'][0]),
        'od_w2_k': f(inputs['od_w2_k'][0]), 'od_pe_v': f(inputs['od_pe_v'][0]), 'od_w1_v': f(inputs['od_w1_v'][0]),
        'od_w2_v': f(inputs['od_w2_v'][0]), 'od_w_out': f(inputs['od_w_out'][0]),
        'xa_w_q': f(inputs['xa_w_q']), 'xa_w_k': f(inputs['xa_w_k']), 'xa_w_v': f(inputs['xa_w_v']), 'xa_w_o': f(inputs['xa_w_o']),
        'ff_w_gate': f(inputs['ff_w_gate'][0]), 'ff_w_up': f(inputs['ff_w_up'][0]), 'ff_w_down': f(inputs['ff_w_down'][0]),
        'moe_w_router': f(inputs['moe_w_router'][0]), 'moe_w_gate': f(inputs['moe_w_gate'][0]),
        'moe_w_up': f(inputs['moe_w_up'][0]), 'moe_w_down': f(inputs['moe_w_down'][0]),
        'ln_g': f(inputs['ln_g']), 'ln_b': f(inputs['ln_b']), 'bt_tab': bt_tab, 'cfar': cfar,
        'cb_tab': cb_tab, 'ovl': ovl, 'eblk': eblk, 'gsel': gsel, 'seltab': np.ascontiguousarray(seltab),
    }
    return shared


def run(inputs, phases, cores=NCORES):
    key = tuple(phases)
    if key not in _PROG_CACHE:
        _PROG_CACHE[key] = Prog(phases)
    prog = _PROG_CACHE[key]
    shared = make_inputs(inputs)
    x = np.asarray(inputs['x'], dtype=np.float32)
    mem = np.asarray(inputs['mem'], dtype=np.float32)
    in_maps = []
    for c in range(cores):
        m = dict(shared)
        m['x'] = np.ascontiguousarray(x[c * NB:(c + 1) * NB])
        m['mem'] = np.ascontiguousarray(mem[c * NB:(c + 1) * NB])
        in_maps.append(m)
    res = run_bass_kernel_spmd(prog.nc, in_maps, core_ids=list(range(cores)))
    return np.concatenate([r['out'] for r in res.results], axis=0)


def kernel(**inputs):
    return run(inputs, FULL_PHASES)
```
